# Optimizing a Trainium2 kernel written in Bass

```python
import jax, jax.numpy as jnp
from jax import lax
import numpy as np


D_MODEL = 4096
BATCH = 2
SEQ = 8192
DEPTH = 1

GRID_W = 64
ATTN_HEADS = 16
HEAD_DIM = 128
ATTN_WIDTH = ATTN_HEADS * HEAD_DIM
WIN_ROWS = 8
WIN_COLS = 16
CONV_WIDTH = 2048
CONV_GROUPS = 16
CONV_K = 3
N_BRANCHES = 2
IN_COLS = 3 * ATTN_WIDTH + 3 * CONV_WIDTH + N_BRANCHES * D_MODEL
PEER_HEADS = 8
PEER_KEY_DIM = 256
N_KEYS = 128
N_EXPERTS = N_KEYS * N_KEYS
PEER_TOPK = 16
PEER_CHUNK = 128
RMS_EPS = 1e-6
N_MOD = 6

kernel_name = 'hybrid_na2d_shortconv_peer_block'


def rmsnorm(x, g):
    xf = x.astype(jnp.float32)
    y = xf * lax.rsqrt(jnp.mean(xf * xf, axis=-1, keepdims=True) + RMS_EPS)
    return (y * g.astype(jnp.float32)).astype(x.dtype)


def modulate(h, shift, scale):
    return h * (1 + scale[:, None, :]) + shift[:, None, :]


def neighbourhood_attention(q, k, v, rpb):
    b, s, h, d = q.shape
    rows = s // GRID_W
    kh = min(WIN_ROWS, rows)
    kg = k.reshape(b, rows, GRID_W, h, d).transpose(0, 3, 1, 2, 4)
    vg = v.reshape(b, rows, GRID_W, h, d).transpose(0, 3, 1, 2, 4)
    qg = q.reshape(b, rows, GRID_W, h, d).transpose(1, 0, 3, 2, 4)
    cols = jnp.arange(GRID_W)
    col_start = jnp.clip(cols - WIN_COLS // 2, 0, GRID_W - WIN_COLS)
    col_idx = col_start[:, None] + jnp.arange(WIN_COLS)[None, :]
    dc = col_idx - cols[:, None] + (WIN_COLS - 1)
    scale = HEAD_DIM ** -0.5

    def row_step(args):
        r, q_row = args
        rs = jnp.clip(r - kh // 2, 0, rows - kh)
        k_rows = lax.dynamic_slice_in_dim(kg, rs, kh, axis=2)
        v_rows = lax.dynamic_slice_in_dim(vg, rs, kh, axis=2)
        k_win = k_rows[:, :, :, col_idx, :]
        v_win = v_rows[:, :, :, col_idx, :]
        dr = rs + jnp.arange(kh) - r + (WIN_ROWS - 1)
        bias = rpb[:, dr[None, :, None], dc[:, None, :]]
        sc = jnp.einsum('bhqd,bhiqjd->bhqij', q_row, k_win).astype(jnp.float32) * scale
        sc = sc + bias.astype(jnp.float32)[None]
        p = jax.nn.softmax(sc.reshape(b, h, GRID_W, kh * WIN_COLS), axis=-1)
        p = p.reshape(b, h, GRID_W, kh, WIN_COLS).astype(v.dtype)
        return jnp.einsum('bhqij,bhiqjd->bhqd', p, v_win)

    out = lax.map(row_step, (jnp.arange(rows), qg))
    return out.transpose(1, 0, 3, 2, 4).reshape(b, s, h * d)


def short_conv_mixer(hin, gate_b, gate_c, conv_w):
    u = gate_c * hin
    y = lax.conv_general_dilated(
        u, conv_w[:, None, :], window_strides=(1,),
        padding=[(CONV_K // 2, CONV_K // 2)],
        dimension_numbers=('NWC', 'WIO', 'NWC'),
        feature_group_count=u.shape[-1])
    return gate_b * y


def peer(h, w_q, sub_keys_1, sub_keys_2, expert_u, expert_v):
    b, s, dm = h.shape
    n_tok = b * s
    t = h.reshape(n_tok, dm)
    q = (t @ w_q).reshape(n_tok, PEER_HEADS, 2, PEER_KEY_DIM // 2)
    s1 = jnp.einsum('thd,hkd->thk', q[:, :, 0], sub_keys_1)
    s2 = jnp.einsum('thd,hkd->thk', q[:, :, 1], sub_keys_2)
    v1, i1 = lax.top_k(s1, PEER_TOPK)
    v2, i2 = lax.top_k(s2, PEER_TOPK)
    cand = (v1[..., :, None] + v2[..., None, :]).reshape(n_tok, PEER_HEADS, PEER_TOPK * PEER_TOPK)
    cand_idx = (i1[..., :, None] * N_KEYS + i2[..., None, :]).reshape(n_tok, PEER_HEADS, PEER_TOPK * PEER_TOPK)
    top_s, pos = lax.top_k(cand, PEER_TOPK)
    idx = jnp.take_along_axis(cand_idx, pos, axis=-1)
    g = jax.nn.softmax(top_s.astype(jnp.float32), axis=-1).astype(h.dtype)
    n_chunks = n_tok // PEER_CHUNK

    def chunk_step(args):
        tc, ic, gc = args
        u = jnp.take(expert_u, ic, axis=0)
        vv = jnp.take(expert_v, ic, axis=0)
        a = jax.nn.gelu(jnp.einsum('cd,ced->ce', tc, u))
        return jnp.einsum('ce,ced->cd', gc * a, vv)

    out = lax.map(chunk_step, (t.reshape(n_chunks, PEER_CHUNK, dm),
                               idx.reshape(n_chunks, PEER_CHUNK, PEER_HEADS * PEER_TOPK),
                               g.reshape(n_chunks, PEER_CHUNK, PEER_HEADS * PEER_TOPK)))
    return out.reshape(b, s, dm)


def setup_inputs(seed: int = 0) -> dict:
    key = jax.random.key(seed)
    ks = jax.random.split(key, 18)
    f32 = jnp.float32

    def nrm(k, shape, s):
        return jax.random.normal(k, shape, f32) * s

    return {
        'x': nrm(ks[0], (BATCH, SEQ, D_MODEL), 1.0),
        'c': nrm(ks[1], (BATCH, D_MODEL), 1.0),
        'w_ada': nrm(ks[2], (DEPTH, D_MODEL, N_MOD * D_MODEL), 0.5 * D_MODEL ** -0.5),
        'b_ada': nrm(ks[3], (DEPTH, N_MOD * D_MODEL), 0.01),
        'norm1_g': 1.0 + nrm(ks[4], (DEPTH, D_MODEL), 0.01),
        'w_in': nrm(ks[5], (DEPTH, D_MODEL, IN_COLS), D_MODEL ** -0.5),
        'rpb': nrm(ks[6], (DEPTH, ATTN_HEADS, 2 * WIN_ROWS - 1, 2 * WIN_COLS - 1), 0.5),
        'conv_w': nrm(ks[7], (DEPTH, CONV_K, CONV_WIDTH), CONV_K ** -0.5),
        'w_attn_out': nrm(ks[8], (DEPTH, ATTN_WIDTH, D_MODEL), ATTN_WIDTH ** -0.5),
        'w_conv_out': nrm(ks[9], (DEPTH, CONV_WIDTH, D_MODEL), CONV_WIDTH ** -0.5),
        'w_o': nrm(ks[10], (DEPTH, D_MODEL, D_MODEL), D_MODEL ** -0.5),
        'norm2_g': 1.0 + nrm(ks[11], (DEPTH, D_MODEL), 0.01),
        'w_q_peer': nrm(ks[12], (DEPTH, D_MODEL, PEER_HEADS * PEER_KEY_DIM), D_MODEL ** -0.5),
        'sub_keys_1': nrm(ks[13], (DEPTH, PEER_HEADS, N_KEYS, PEER_KEY_DIM // 2), (PEER_KEY_DIM // 2) ** -0.5),
        'sub_keys_2': nrm(ks[14], (DEPTH, PEER_HEADS, N_KEYS, PEER_KEY_DIM // 2), (PEER_KEY_DIM // 2) ** -0.5),
        'expert_u': nrm(ks[15], (DEPTH, N_EXPERTS, D_MODEL), D_MODEL ** -0.5),
        'expert_v': nrm(ks[16], (DEPTH, N_EXPERTS, D_MODEL), PEER_HEADS ** -0.5),
        'norm_f_g': 1.0 + nrm(ks[17], (D_MODEL,), 0.01),
    }


def reference(x, c, w_ada, b_ada, norm1_g, w_in, rpb, conv_w, w_attn_out, w_conv_out,
              w_o, norm2_g, w_q_peer, sub_keys_1, sub_keys_2, expert_u, expert_v, norm_f_g):
    b, s, _ = x.shape
    splits = np.cumsum([ATTN_WIDTH, ATTN_WIDTH, ATTN_WIDTH,
                        CONV_WIDTH, CONV_WIDTH, CONV_WIDTH, D_MODEL]).tolist()
    for l in range(DEPTH):
        mod = c @ w_ada[l] + b_ada[l]
        shift1, scale1, gate1, shift2, scale2, gate2 = jnp.split(mod, N_MOD, axis=-1)

        h1 = modulate(rmsnorm(x, norm1_g[l]), shift1, scale1)
        proj = h1 @ w_in[l]
        q, k, v, cb, cc, ch, ga, gb = jnp.split(proj, splits, axis=-1)
        hs = (b, s, ATTN_HEADS, HEAD_DIM)
        attn = neighbourhood_attention(q.reshape(hs), k.reshape(hs), v.reshape(hs), rpb[l])
        y_a = attn @ w_attn_out[l]
        y_b = short_conv_mixer(ch, cb, cc, conv_w[l]) @ w_conv_out[l]
        merged = jax.nn.sigmoid(ga) * y_a + jax.nn.sigmoid(gb) * y_b
        x = x + gate1[:, None, :] * (merged @ w_o[l])

        h2 = modulate(rmsnorm(x, norm2_g[l]), shift2, scale2)
        x = x + gate2[:, None, :] * peer(h2, w_q_peer[l], sub_keys_1[l], sub_keys_2[l],
                                         expert_u[l], expert_v[l])
    return rmsnorm(x, norm_f_g)
```

```python
import numpy as np
import concourse.bass as bass
import concourse.mybir as mybir
from concourse.bass_utils import run_bass_kernel_spmd
from contextlib import ExitStack

F32 = mybir.dt.float32
F32R = mybir.dt.float32r
AF = mybir.ActivationFunctionType
ALU = mybir.AluOpType
AX = mybir.AxisListType

D = 4096
NTOK = 2048
HALO = 256
NEXT = NTOK + 2 * HALO
INC = 20480
NEG = -1.0e30
EPS = 1e-6
SCALE = 128 ** -0.5


class Res:
    __slots__ = ("name", "w", "r", "key")

    def __init__(self, name, key=None):
        self.name = name
        self.w = {}
        self.r = {}
        self.key = key if key is not None else name


class Op:
    __slots__ = ("eng", "fn", "deps", "need", "val", "dma_key", "idx")


class Sched:
    ENG = ("pe", "act", "dve", "pool", "sp")

    def __init__(self, nc):
        self.nc = nc
        self.ops = {e: [] for e in self.ENG}
        self.dma_cnt = {}
        self.n = 0
        self.strict = True

    def op(self, eng, fn, reads=(), writes=(), dma_key=None):
        o = Op()
        o.eng = eng
        o.fn = fn
        o.need = False
        o.val = None
        o.dma_key = dma_key
        o.idx = self.n
        self.n += 1
        deps = {}
        for r in reads:
            for d in r.w.values():
                deps[id(d)] = d
        for w in writes:
            for d in w.w.values():
                deps[id(d)] = d
            for d in w.r.values():
                deps[id(d)] = d
        dl = []
        for d in deps.values():
            if d.dma_key is None and d.eng == eng and dma_key is None and (eng == "pe" or not self.strict):
                continue
            if dma_key is not None and d.dma_key == dma_key:
                continue
            d.need = True
            dl.append(d)
        o.deps = dl
        if dma_key is not None:
            c = self.dma_cnt.get(dma_key, 0) + 1
            self.dma_cnt[dma_key] = c
            o.val = 16 * c
            key = "dma:" + dma_key
        else:
            key = eng
        for r in reads:
            r.r[key] = o
        for w in writes:
            w.w = {key: o}
            w.r = {}
        self.ops[eng].append(o)
        return o

    def barrier(self):
        lasts = []
        for e in self.ENG:
            for o_ in reversed(self.ops[e]):
                if o_.fn is not None and o_.dma_key is None:
                    lasts.append(o_)
                    break
        dmas = dict(self.dma_cnt)
        for e in self.ENG:
            o = Op()
            o.eng = e
            o.fn = None
            o.need = False
            o.val = None
            o.dma_key = None
            o.idx = self.n
            self.n += 1
            dl = []
            for l in lasts:
                if l.eng != e and l.dma_key is None:
                    l.need = True
                    dl.append(l)
            o.deps = dl + [("dma", k, 16 * c) for k, c in dmas.items()]
            self.ops[e].append(o)

    def emit(self):
        nc = self.nc
        for e in self.ENG:
            c = 0
            for o in self.ops[e]:
                if o.dma_key is None and o.need:
                    c += 1
                    o.val = c
        sems = {}

        def sem(k):
            if k not in sems:
                sems[k] = nc.alloc_semaphore(name="s_" + k.replace(":", "_"))
            return sems[k]

        handles = {"pe": nc.tensor, "act": nc.scalar, "dve": nc.vector, "pool": nc.gpsimd, "sp": nc.sync}

        def run(e, h):
            waited = {}
            for o in self.ops[e]:
                for d in o.deps:
                    if isinstance(d, tuple):
                        k, v = "dma:" + d[1], d[2]
                    elif d.dma_key is not None:
                        k, v = "dma:" + d.dma_key, d.val
                    else:
                        k, v = d.eng, d.val
                    if waited.get(k, 0) < v:
                        h.wait_ge(sem(k), v)
                        waited[k] = v
                if o.fn is None:
                    continue
                ins = o.fn(h)
                if o.dma_key is not None:
                    ins.then_inc(sem("dma:" + o.dma_key), 16)
                elif o.need:
                    ins.then_inc(sem(e), 1)

        with nc.Block() as block:
            @block.tensor
            def _(h):
                run("pe", h)

            @block.scalar
            def _(h):
                run("act", h)

            @block.vector
            def _(h):
                run("dve", h)

            @block.gpsimd
            def _(h):
                run("pool", h)

            @block.sync
            def _(h):
                run("sp", h)

    def dma(self, out, in_, key, reads=(), writes=(), q="sp"):
        return self.op(q, lambda h: h.dma_start(out=out, in_=in_), reads, writes, dma_key=key)

    def mm(self, out, lhsT, rhs, start, stop, reads=(), writes=()):
        return self.op("pe", lambda h: h.matmul(out, lhsT, rhs, start=start, stop=stop, skip_group_check=True), reads, writes)

    def tr(self, out, in_, ident, reads=(), writes=()):
        return self.op("pe", lambda h: h.transpose(out, in_, ident), reads, writes)

    def act(self, out, in_, func, reads=(), writes=(), eng="act", **kw):
        return self.op(eng, lambda h: h.activation(out=out, in_=in_, func=func, **kw), reads, writes)

    def tt(self, out, in0, in1, op, reads=(), writes=(), eng="dve"):
        return self.op(eng, lambda h: h.tensor_tensor(out=out, in0=in0, in1=in1, op=op), reads, writes)

    def ts(self, out, in0, s1, s2, op0, op1=None, reads=(), writes=(), eng="dve"):
        if op1 is None:
            return self.op(eng, lambda h: h.tensor_scalar(out=out, in0=in0, scalar1=s1, scalar2=None, op0=op0), reads, writes)
        return self.op(eng, lambda h: h.tensor_scalar(out=out, in0=in0, scalar1=s1, scalar2=s2, op0=op0, op1=op1), reads, writes)

    def stt(self, out, in0, scalar, in1, op0, op1, reads=(), writes=()):
        return self.op("dve", lambda h: h.scalar_tensor_tensor(out=out, in0=in0, scalar=scalar, in1=in1, op0=op0, op1=op1), reads, writes)

    def cp(self, out, in_, reads=(), writes=(), eng="dve"):
        if eng == "act":
            return self.op("act", lambda h: h.copy(out=out, in_=in_), reads, writes)
        return self.op(eng, lambda h: h.tensor_copy(out=out, in_=in_), reads, writes)

    def rmax(self, out, in_, reads=(), writes=()):
        return self.op("dve", lambda h: h.reduce_max(out=out, in_=in_, axis=AX.X), reads, writes)

    def rsum(self, out, in_, reads=(), writes=()):
        return self.op("dve", lambda h: h.reduce_sum(out=out, in_=in_, axis=AX.X), reads, writes)

    def recip(self, out, in_, reads=(), writes=()):
        return self.op("dve", lambda h: h.reciprocal(out=out, in_=in_), reads, writes)

    def max8(self, out, in_, reads=(), writes=()):
        return self.op("dve", lambda h: h.max(out=out, in_=in_), reads, writes)

    def mrep(self, out, rep, vals, reads=(), writes=()):
        return self.op("dve", lambda h: h.match_replace(out=out, in_to_replace=rep, in_values=vals, imm_value=NEG), reads, writes)

    def gen(self, eng, fn, reads=(), writes=()):
        return self.op(eng, fn, reads, writes)


def build_nc(upto=99, debug=False):
    nc = bass.Bass("TRN2", target_bir_lowering=False)
    nc.dge_precook = False
    S = Sched(nc)
    skind = "ExternalOutput" if debug else "Internal"

    def din(name, shape, dt=F32):
        return nc.dram_tensor(name, list(shape), dt, kind="ExternalInput").ap()

    xT = din("xT", [D, NEXT])
    cT = din("cT", [128, 32], F32R)
    w_ada = din("w_ada", [D, 6 * D], F32R)
    b_ada = din("b_ada", [1, 6 * D])
    g1 = din("g1", [128, 32])
    g2 = din("g2", [128, 32])
    gf = din("gf", [128, 32])
    w_in = din("w_in", [80, 128, 32 * 256], F32R)
    biasg = din("biasg", [16, 128, 640])
    bias1 = din("bias1", [16, 128, 640])
    bias14 = din("bias14", [16, 128, 640])
    biast = din("biast", [16, 128, 768])
    biasb = din("biasb", [16, 128, 768])
    convw = din("convw", [128, 16, 3])
    w_ao = din("w_ao", [16, 128, 16 * 256], F32R)
    w_co = din("w_co", [16, 128, 16 * 256], F32R)
    w_o = din("w_o", [16, 128, 32 * 256], F32R)
    w_q = din("w_q", [16, 128, 32 * 128], F32R)
    keysT = din("keysT", [128, 16, 128])
    UT = din("UT", [128, 128, 32 * 128], F32R)
    EV = din("EV", [16, 8, 4, 128, 2 * 512], F32R)
    ident_d = din("ident", [128, 128])
    flags_d = din("flags", [128, 2])
    outT = nc.dram_tensor("outT", [D, NTOK], F32, kind="ExternalOutput").ap()
    featT = nc.dram_tensor("featT", [INC, NEXT], F32R, kind=skind).ap()
    vtok = nc.dram_tensor("vtok", [NEXT, 2048], F32R, kind=skind).ap()
    attnT = nc.dram_tensor("attnT", [2048, NTOK], F32R, kind=skind).ap()
    x1T = nc.dram_tensor("x1T", [D, NTOK], F32, kind=skind).ap()
    modd = nc.dram_tensor("modd", [128, 6 * 32], F32, kind=skind).ap()

    sb = nc.alloc_sbuf_tensor
    mod = sb("mod", [128, 6, 32], F32)
    gs1 = sb("gs1", [128, 32], F32)
    gs2 = sb("gs2", [128, 32], F32)
    g1s = sb("g1s", [128, 32], F32)
    g2s = sb("g2s", [128, 32], F32)
    gfs = sb("gfs", [128, 32], F32)
    ones = sb("ones", [128, 128], F32R)
    ident = sb("identsb", [128, 128], F32)
    flags = sb("flagssb", [128, 2], F32)
    one1 = sb("one1", [1, 1], F32)
    R_const = Res("const")
    ps = [nc.alloc_psum_tensor("ps%d" % i, [128, 512], F32) for i in range(8)]
    RP = [Res("ps%d" % i) for i in range(8)]

    S.dma(g1s[:], g1, "const", writes=[R_const])
    S.dma(g2s[:], g2, "const", writes=[R_const])
    S.dma(gfs[:], gf, "const", writes=[R_const])
    S.dma(ident[:], ident_d, "const", writes=[R_const])
    S.dma(flags[:], flags_d, "const", writes=[R_const])
    onesf = sb("onesf", [128, 128], F32)
    S.gen("dve", lambda h: h.memset(onesf[:], 1.0), writes=[R_const])
    S.cp(ones[:], onesf[:], reads=[R_const], writes=[R_const])
    S.gen("dve", lambda h: h.memset(one1[:], 1.0), writes=[R_const])

    es0 = ExitStack()
    sb0 = lambda n, sh, dt: es0.enter_context(nc.sbuf_tensor(n, sh, dt))
    cTs = sb0("cTs", [128, 32], F32R)
    was = [sb0("wa%d" % i, [128, 4096], F32R) for i in range(3)]
    RWA = [Res("wa%d" % i) for i in range(3)]
    rowb = sb0("rowb", [1, 4096], F32)
    row = sb0("row", [1, 4096], F32)
    R_cT, R_rowb, R_row, R_mod = Res("cT"), Res("rowb"), Res("row"), Res("mod")
    S.dma(cTs[:], cT, "cT", writes=[R_cT])
    li = 0
    for g in range(6):
        S.dma(rowb[:], b_ada[:, g * 4096:(g + 1) * 4096], "rowb", writes=[R_rowb])
        for k in range(32):
            sl = li % 3
            li += 1
            for hh in range(2):
                S.dma(was[sl][:, hh * 2048:(hh + 1) * 2048],
                      w_ada[k * 128:(k + 1) * 128, g * 4096 + hh * 2048: g * 4096 + (hh + 1) * 2048],
                      "wa%d" % sl, writes=[RWA[sl]])
            for b in range(8):
                S.mm(ps[b][0:1, :], cTs[:, k:k + 1], was[sl][:, b * 512:(b + 1) * 512], k == 0, k == 31,
                     reads=[R_cT, RWA[sl]], writes=[RP[b]])
        for b in range(8):
            S.tt(row[:, b * 512:(b + 1) * 512], ps[b][0:1, :], rowb[:, b * 512:(b + 1) * 512], ALU.add,
                 reads=[RP[b], R_rowb], writes=[R_row])
        for j in range(32):
            S.mm(ps[0][:, j:j + 1], row[0:1, j * 128:(j + 1) * 128], one1[0:1, 0:1], True, True,
                 reads=[R_row, R_const], writes=[RP[0]])
        S.cp(mod[:, g, :], ps[0][:, 0:32], reads=[RP[0]], writes=[R_mod])
    S.stt(gs1[:], mod[:, 1, :], 1.0, g1s[:], ALU.add, ALU.mult, reads=[R_mod, R_const], writes=[R_mod])
    S.stt(gs2[:], mod[:, 4, :], 1.0, g2s[:], ALU.add, ALU.mult, reads=[R_mod, R_const], writes=[R_mod])
    if debug:
        S.dma(modd, mod[:].rearrange("p a b -> p (a b)"), "modd", reads=[R_mod])
    sh1, gate1, sh2, gate2 = mod[:, 0, :], mod[:, 2, :], mod[:, 3, :], mod[:, 5, :]
    S.barrier()
    es0.close()
    if upto < 1:
        S.emit()
        return nc

    def norm_tile(src_load, T, dst, gs, sh, nk=32):
        for kq in range(8):
            sl = kq % 2
            src_load(kq, xs[sl], RXS[sl])
            for kk in range(4):
                k = kq * 4 + kk
                q = k % 2
                S.act(sq[q][:, 0:T], xs[sl][:, kk, 0:T], AF.Square, reads=[RXS[sl]], writes=[RSQ[q]])
                S.mm(ps[0][:, 0:T], ones[:], sq[q][:, 0:T], k == 0, k == 31, reads=[RSQ[q], R_const], writes=[RP[0]])
        S.ts(rstd[:, 0:T], ps[0][:, 0:T], 1.0 / D, EPS, ALU.mult, ALU.add, reads=[RP[0]], writes=[R_rstd])
        S.act(rstd[:, 0:T], rstd[:, 0:T], AF.Sqrt, reads=[R_rstd], writes=[R_rstd])
        S.recip(rstd[:, 0:T], rstd[:, 0:T], reads=[R_rstd], writes=[R_rstd])
        for kq in range(8):
            sl = kq % 2
            src_load(kq, xs[sl], RXS[sl])
            for kk in range(4):
                k = kq * 4 + kk
                S.tt(xs[sl][:, kk, 0:T], xs[sl][:, kk, 0:T], rstd[:, 0:T], ALU.mult, reads=[RXS[sl], R_rstd], writes=[RXS[sl]], eng="pool")
                S.ts(dst[:, k, 0:T], xs[sl][:, kk, 0:T], gs[:, k:k + 1], sh[:, k:k + 1], ALU.mult, ALU.add,
                     reads=[RXS[sl], R_mod], writes=[R_dst[0]])

    esA = ExitStack()
    sb = lambda n, sh, dt: esA.enter_context(nc.sbuf_tensor(n, sh, dt))
    xs = [sb("xs%d" % i, [128, 4, 512], F32) for i in range(2)]
    RXS = [Res("xs%d" % i) for i in range(2)]
    sq = [sb("sq%d" % i, [128, 512], F32R) for i in range(2)]
    RSQ = [Res("sq%d" % i) for i in range(2)]
    rstd = sb("rstd", [128, 512], F32)
    R_rstd = Res("rstd")
    hT = sb("hT", [128, 32, 512], F32R)
    R_hT = Res("hT")
    R_dst = [R_hT]
    ws = [sb("ws%d" % i, [128, 32, 256], F32R) for i in range(3)]
    RWS = [Res("ws%d" % i) for i in range(3)]
    stg = [sb("stg%d" % i, [128, 512], F32R) for i in range(4)]
    RSTG = [Res("stgA%d" % i) for i in range(4)]
    xTv = xT.rearrange("(k p) t -> p k t", p=128)
    vtokv = vtok
    wi = 0
    si = 0
    pi = 0
    for ti in range(5):
        if ti < 4:
            segs = [(HALO + 512 * ti, 0, 512)]
        else:
            segs = [(0, 0, 256), (NTOK + HALO, 256, 256)]

        def src_load(kq, slot, res, segs=segs):
            for (e0, o0, ln) in segs:
                S.dma(slot[:, :, o0:o0 + ln], xTv[:, kq * 4:(kq + 1) * 4, e0:e0 + ln], res.key, writes=[res])
        norm_tile(src_load, 512, hT, gs1, sh1)
        if ti < 4:
            cslots = list(range(80))
        else:
            cslots = list(range(8, 24)) + list(range(32, 48))
        for cs in cslots:
            c0 = cs * 256
            sl = wi % 3
            wi += 1
            for kq in range(2):
                S.dma(ws[sl][:, kq * 16:(kq + 1) * 16, :],
                      w_in[cs][:, kq * 4096:(kq + 1) * 4096].rearrange("p (k n) -> p k n", n=256), "ws%d" % sl, writes=[RWS[sl]])
            isv = 4096 <= c0 < 6144
            if isv:
                for sub in range(4):
                    b = 1 + pi % 4
                    pi += 1
                    for k in range(32):
                        S.mm(ps[b][:, 0:256], hT[:, k, sub * 128:(sub + 1) * 128], ws[sl][:, k, :], k == 0, k == 31,
                             reads=[R_hT, RWS[sl]], writes=[RP[b]])
                    st = si % 4
                    si += 1
                    S.cp(stg[st][:, 0:256], ps[b][:, 0:256], reads=[RP[b]], writes=[RSTG[st]], eng="act")
                    o = sub * 128
                    for (e0, o0, ln) in segs:
                        if o0 <= o < o0 + ln:
                            S.dma(vtokv[e0 + o - o0: e0 + o - o0 + 128, c0 - 4096: c0 - 4096 + 256], stg[st][:, 0:256],
                                  RSTG[st].key, reads=[RSTG[st]], q="act")
            else:
                for half in range(2):
                    b = 1 + pi % 4
                    pi += 1
                    for k in range(32):
                        S.mm(ps[b][:], ws[sl][:, k, half * 128:(half + 1) * 128], hT[:, k, :], k == 0, k == 31,
                             reads=[R_hT, RWS[sl]], writes=[RP[b]])
                    st = si % 4
                    si += 1
                    S.cp(stg[st][:], ps[b][:], reads=[RP[b]], writes=[RSTG[st]], eng="act")
                    r0 = c0 + half * 128
                    for (e0, o0, ln) in segs:
                        S.dma(featT[r0:r0 + 128, e0:e0 + ln], stg[st][:, o0:o0 + ln], RSTG[st].key, reads=[RSTG[st]], q="act")
    S.barrier()
    esA.close()
    if upto < 2:
        S.emit()
        return nc

    esB = ExitStack()
    sb = lambda n, sh, dt: esB.enter_context(nc.sbuf_tensor(n, sh, dt))
    qh = [sb("qh%d" % i, [128, 2048], F32R) for i in range(2)]
    kh = [sb("kh%d" % i, [128, NEXT], F32R) for i in range(2)]
    vh = [sb("vh%d" % i, [128, 20, 128], F32R) for i in range(2)]
    bgs = [sb("bg%d" % i, [128, 640], F32) for i in range(2)]
    b1s = [sb("b1%d" % i, [128, 640], F32) for i in range(2)]
    b14s = [sb("b14%d" % i, [128, 640], F32) for i in range(2)]
    bts = [sb("bt%d" % i, [128, 768], F32) for i in range(2)]
    bbs = [sb("bb%d" % i, [128, 768], F32) for i in range(2)]
    RQ = [Res("qh%d" % i) for i in range(2)]
    RK = [Res("kh%d" % i) for i in range(2)]
    RV = [Res("vh%d" % i) for i in range(2)]
    RB = [Res("bias%d" % i) for i in range(2)]
    sbt = [sb("sbt%d" % i, [128, 768], F32) for i in range(2)]
    Pn = [sb("Pn%d" % i, [128, 768], F32) for i in range(2)]
    PTs = [sb("PTs%d" % i, [128, 768], F32R) for i in range(2)]
    smx = [sb("smx%d" % i, [128, 4], F32) for i in range(2)]
    ah = [sb("ah%d" % i, [128, 2048], F32R) for i in range(2)]
    R_sbt = [Res("sbt%d" % i) for i in range(2)]
    R_Pn = [Res("Pn%d" % i) for i in range(2)]
    R_PT = [Res("PTs%d" % i) for i in range(2)]
    R_sm = [Res("smx%d" % i) for i in range(2)]
    RAH = [Res("ah%d" % i) for i in range(2)]
    vtv = vtok.rearrange("(n p) c -> p n c", p=128)
    it = 0
    for h in range(16):
        p = h % 2
        S.dma(qh[p][:], featT[h * 128:(h + 1) * 128, HALO:HALO + NTOK], RQ[p].key, writes=[RQ[p]])
        S.dma(kh[p][:], featT[2048 + h * 128:2048 + (h + 1) * 128, :], RK[p].key, writes=[RK[p]])
        S.dma(vh[p][:, 0:10, :], vtv[:, 0:10, h * 128:(h + 1) * 128], RV[p].key, writes=[RV[p]])
        S.dma(vh[p][:, 10:20, :], vtv[:, 10:20, h * 128:(h + 1) * 128], RV[p].key, writes=[RV[p]])
        S.dma(bgs[p][:], biasg[h], RB[p].key, writes=[RB[p]])
        S.dma(b1s[p][:], bias1[h], RB[p].key, writes=[RB[p]])
        S.dma(b14s[p][:], bias14[h], RB[p].key, writes=[RB[p]])
        S.dma(bts[p][:], biast[h], RB[p].key, writes=[RB[p]])
        S.dma(bbs[p][:], biasb[h], RB[p].key, writes=[RB[p]])
        for j in range(16):
            if j == 0:
                tiles, bias = list(range(0, 6)), bts[p]
            elif j == 15:
                tiles, bias = list(range(14, 20)), bbs[p]
            elif j == 1:
                tiles, bias = list(range(1, 6)), b1s[p]
            elif j == 14:
                tiles, bias = list(range(14, 19)), b14s[p]
            else:
                tiles, bias = list(range(j, j + 5)), bgs[p]
            nt = len(tiles)
            W = nt * 128
            e0 = tiles[0] * 128
            d = it % 2
            it += 1
            b0, b1_ = 2 + 2 * d, 3 + 2 * d
            qsl = qh[p][:, j * 128:(j + 1) * 128]
            S.mm(ps[b0][:, 0:512], qsl, kh[p][:, e0:e0 + 512], True, True, reads=[RQ[p], RK[p]], writes=[RP[b0]])
            S.mm(ps[b1_][:, 0:W - 512], qsl, kh[p][:, e0 + 512:e0 + W], True, True, reads=[RQ[p], RK[p]], writes=[RP[b1_]])
            S.stt(sbt[d][:, 0:512], ps[b0][:, 0:512], SCALE, bias[:, 0:512], ALU.mult, ALU.add,
                  reads=[RP[b0], RB[p]], writes=[R_sbt[d]])
            S.stt(sbt[d][:, 512:W], ps[b1_][:, 0:W - 512], SCALE, bias[:, 512:W], ALU.mult, ALU.add,
                  reads=[RP[b1_], RB[p]], writes=[R_sbt[d]])
            S.rmax(smx[d][:, 0:1], sbt[d][:, 0:W], reads=[R_sbt[d]], writes=[R_sm[d]])
            S.ts(smx[d][:, 1:2], smx[d][:, 0:1], -1.0, None, ALU.mult, reads=[R_sm[d]], writes=[R_sm[d]])
            S.act(Pn[d][:, 0:W], sbt[d][:, 0:W], AF.Exp, bias=smx[d][:, 1:2], accum_out=smx[d][:, 2:3],
                  reads=[R_sbt[d], R_sm[d]], writes=[R_Pn[d], R_sm[d]])
            S.recip(smx[d][:, 3:4], smx[d][:, 2:3], reads=[R_sm[d]], writes=[R_sm[d]])
            S.ts(Pn[d][:, 0:W], Pn[d][:, 0:W], smx[d][:, 3:4], None, ALU.mult, reads=[R_Pn[d], R_sm[d]], writes=[R_Pn[d]])
            for i in range(nt):
                bank = 6 if i < 4 else 7
                off = (i % 4) * 128
                S.tr(ps[bank][:, off:off + 128], Pn[d][:, i * 128:(i + 1) * 128], ident[:], reads=[R_Pn[d], R_const], writes=[RP[bank]])
            S.cp(PTs[d][:, 0:512], ps[6][:, 0:512], reads=[RP[6]], writes=[R_PT[d]], eng="act")
            S.cp(PTs[d][:, 512:W], ps[7][:, 0:W - 512], reads=[RP[7]], writes=[R_PT[d]], eng="act")
            for i in range(nt):
                S.mm(ps[1][:, 0:128], vh[p][:, tiles[i], :], PTs[d][:, i * 128:(i + 1) * 128], i == 0, i == nt - 1,
                     reads=[RV[p], R_PT[d]], writes=[RP[1]])
            S.cp(ah[p][:, j * 128:(j + 1) * 128], ps[1][:, 0:128], reads=[RP[1]], writes=[RAH[p]], eng="dve")
        S.dma(attnT[h * 128:(h + 1) * 128, :], ah[p][:], RAH[p].key, reads=[RAH[p]], q="act")
    S.barrier()
    esB.close()
    if upto < 3:
        S.emit()
        return nc

    esC = ExitStack()
    sb = lambda n, sh, dt: esC.enter_context(nc.sbuf_tensor(n, sh, dt))
    TB = 256
    aT = sb("aT", [128, 16, TB], F32R)
    cvT = sb("cvT", [128, 16, TB], F32R)
    mg = sb("mg", [128, 32, TB], F32R)
    wsl = [sb("wsl%d" % i, [128, 32, 256], F32R) for i in range(2)]
    RWS2 = [Res("wsl%d" % i) for i in range(2)]
    R_aT, R_cvT, R_mg = Res("aT"), Res("cvT"), Res("mg")
    cw = sb("cw", [128, 16, 3], F32)
    R_cw = Res("cw")
    S.dma(cw[:], convw, "cw", writes=[R_cw])
    cct = [sb("cct%d" % i, [128, TB + 2], F32) for i in range(2)]
    cht = [sb("cht%d" % i, [128, TB + 2], F32) for i in range(2)]
    cbt = [sb("cbt%d" % i, [128, TB], F32) for i in range(2)]
    ut = [sb("ut%d" % i, [128, TB + 2], F32) for i in range(2)]
    yt = [sb("yt%d" % i, [128, TB], F32) for i in range(2)]
    RCC = [Res("cct%d" % i) for i in range(2)]
    RCH = [Res("cht%d" % i) for i in range(2)]
    RCB = [Res("cbt%d" % i) for i in range(2)]
    RU = [Res("ut%d" % i) for i in range(2)]
    RY = [Res("yt%d" % i) for i in range(2)]
    gat = [sb("gat%d" % i, [128, TB], F32) for i in range(2)]
    gbt = [sb("gbt%d" % i, [128, TB], F32) for i in range(2)]
    t1 = [sb("t1%d" % i, [128, TB], F32) for i in range(2)]
    t2 = [sb("t2%d" % i, [128, TB], F32) for i in range(2)]
    RGA = [Res("gat%d" % i) for i in range(2)]
    RGB = [Res("gbt%d" % i) for i in range(2)]
    RT1 = [Res("t1%d" % i) for i in range(2)]
    RT2 = [Res("t2%d" % i) for i in range(2)]
    xm = [sb("xm%d" % i, [128, TB], F32) for i in range(2)]
    stg2 = [sb("stgB%d" % i, [128, TB], F32) for i in range(2)]
    RXM = [Res("xm%d" % i) for i in range(2)]
    RST2 = [Res("stgB%d" % i) for i in range(2)]
    attnTv = attnT.rearrange("(k p) t -> p k t", p=128)
    featF = featT.bitcast(F32)
    wi = 0
    pi = 0
    for ti in range(NTOK // TB):
        t0 = ti * TB
        e0 = HALO + t0
        S.dma(aT[:, 0:8, :], attnTv[:, 0:8, t0:t0 + TB], "aT", writes=[R_aT])
        S.dma(aT[:, 8:16, :], attnTv[:, 8:16, t0:t0 + TB], "aT", writes=[R_aT])
        for k in range(16):
            q = k % 2
            S.dma(cct[q][:], featF[8192 + k * 128:8192 + (k + 1) * 128, e0 - 1:e0 + TB + 1], RCC[q].key, writes=[RCC[q]], q="act")
            S.dma(cht[q][:], featF[10240 + k * 128:10240 + (k + 1) * 128, e0 - 1:e0 + TB + 1], RCH[q].key, writes=[RCH[q]], q="act")
            S.dma(cbt[q][:], featF[6144 + k * 128:6144 + (k + 1) * 128, e0:e0 + TB], RCB[q].key, writes=[RCB[q]], q="act")
            S.tt(ut[q][:], cct[q][:], cht[q][:], ALU.mult, reads=[RCC[q], RCH[q]], writes=[RU[q]], eng="pool")
            if ti == 0:
                S.ts(ut[q][:, 0:1], ut[q][:, 0:1], flags[:, 0:1], None, ALU.mult, reads=[RU[q], R_const], writes=[RU[q]], eng="pool")
            if ti == NTOK // TB - 1:
                S.ts(ut[q][:, TB + 1:TB + 2], ut[q][:, TB + 1:TB + 2], flags[:, 1:2], None, ALU.mult, reads=[RU[q], R_const], writes=[RU[q]], eng="pool")
            S.ts(yt[q][:], ut[q][:, 0:TB], cw[:, k, 0:1], None, ALU.mult, reads=[RU[q], R_cw], writes=[RY[q]])
            S.stt(yt[q][:], ut[q][:, 1:TB + 1], cw[:, k, 1:2], yt[q][:], ALU.mult, ALU.add, reads=[RU[q], R_cw, RY[q]], writes=[RY[q]])
            S.stt(yt[q][:], ut[q][:, 2:TB + 2], cw[:, k, 2:3], yt[q][:], ALU.mult, ALU.add, reads=[RU[q], R_cw, RY[q]], writes=[RY[q]])
            S.tt(cvT[:, k, :], cbt[q][:], yt[q][:], ALU.mult, reads=[RCB[q], RY[q]], writes=[R_cvT])
        for cs in range(16):
            c0 = cs * 256
            sl = wi % 2
            wi += 1
            S.dma(wsl[sl][:, 0:16, :], w_ao[cs].rearrange("p (k n) -> p k n", n=256), RWS2[sl].key, writes=[RWS2[sl]])
            S.dma(wsl[sl][:, 16:32, :], w_co[cs].rearrange("p (k n) -> p k n", n=256), RWS2[sl].key, writes=[RWS2[sl]])
            for half in range(2):
                m = cs * 2 + half
                ba, bb_ = (1, 2) if pi % 2 == 0 else (3, 4)
                pi += 1
                for k in range(16):
                    S.mm(ps[ba][:, 0:TB], wsl[sl][:, k, half * 128:(half + 1) * 128], aT[:, k, :], k == 0, k == 15,
                         reads=[RWS2[sl], R_aT], writes=[RP[ba]])
                for k in range(16):
                    S.mm(ps[bb_][:, 0:TB], wsl[sl][:, 16 + k, half * 128:(half + 1) * 128], cvT[:, k, :], k == 0, k == 15,
                         reads=[RWS2[sl], R_cvT], writes=[RP[bb_]])
                q = m % 2
                S.dma(gat[q][:], featF[12288 + m * 128:12288 + (m + 1) * 128, e0:e0 + TB], RGA[q].key, writes=[RGA[q]], q="act")
                S.dma(gbt[q][:], featF[16384 + m * 128:16384 + (m + 1) * 128, e0:e0 + TB], RGB[q].key, writes=[RGB[q]], q="act")
                S.act(gat[q][:], gat[q][:], AF.Sigmoid, reads=[RGA[q]], writes=[RGA[q]])
                S.act(gbt[q][:], gbt[q][:], AF.Sigmoid, reads=[RGB[q]], writes=[RGB[q]])
                S.tt(t1[q][:], ps[ba][:, 0:TB], gat[q][:], ALU.mult, reads=[RP[ba], RGA[q]], writes=[RT1[q]])
                S.tt(t2[q][:], ps[bb_][:, 0:TB], gbt[q][:], ALU.mult, reads=[RP[bb_], RGB[q]], writes=[RT2[q]])
                S.tt(mg[:, m, :], t1[q][:], t2[q][:], ALU.add, reads=[RT1[q], RT2[q]], writes=[R_mg])
        for cs in range(16):
            c0 = cs * 256
            sl = wi % 2
            wi += 1
            for kq in range(2):
                S.dma(wsl[sl][:, kq * 16:(kq + 1) * 16, :],
                      w_o[cs][:, kq * 4096:(kq + 1) * 4096].rearrange("p (k n) -> p k n", n=256), RWS2[sl].key, writes=[RWS2[sl]])
            for half in range(2):
                m = cs * 2 + half
                bo = 5 + m % 2
                for k in range(32):
                    S.mm(ps[bo][:, 0:TB], wsl[sl][:, k, half * 128:(half + 1) * 128], mg[:, k, :], k == 0, k == 31,
                         reads=[RWS2[sl], R_mg], writes=[RP[bo]])
                q = m % 2
                S.dma(xm[q][:], xT[m * 128:(m + 1) * 128, e0:e0 + TB], RXM[q].key, writes=[RXM[q]], q="act")
                S.stt(stg2[q][:], ps[bo][:, 0:TB], gate1[:, m:m + 1], xm[q][:], ALU.mult, ALU.add,
                      reads=[RP[bo], RXM[q], R_mod], writes=[RST2[q]])
                S.dma(x1T[m * 128:(m + 1) * 128, t0:t0 + TB], stg2[q][:], RST2[q].key, reads=[RST2[q]], q="act")
    S.barrier()
    esC.close()
    if upto < 4:
        S.emit()
        return nc

    esD = ExitStack()
    sb = lambda n, sh, dt: esD.enter_context(nc.sbuf_tensor(n, sh, dt))
    TC = 256
    NSB = 16
    accmem = sb("accmem", [128, 32 * TC], F32)
    acc = accmem[:].rearrange("p (k t) -> p k t", k=32)
    P_tok = [accmem[:, sub * 4096:(sub + 1) * 4096] for sub in range(2)]
    acc2 = accmem[:].rearrange("p (s d) -> p s d", s=2)
    xmc = [sb("xmc%d" % i, [128, TC], F32) for i in range(2)]
    RXMC = [Res("xmc%d" % i) for i in range(2)]
    RACC = [Res("acc%d" % m) for m in range(32)]
    h2T = sb("h2T", [128, 32, TC], F32R)
    R_h2 = Res("h2T")
    NUS, NVS = 3, 3
    us = [sb("us%d" % i, [128, 32, 128], F32R) for i in range(NUS)]
    RUS = [Res("us%d" % i) for i in range(NUS)]
    vs_ = [sb("vs%d" % i, [128, 2, 512], F32R) for i in range(NVS)]
    RVS = [Res("vs%d" % i) for i in range(NVS)]
    NTILE_C = NTOK // 256
    ust = {"next": 0, "use": 0}
    vst = {"next": 0, "use": 0}

    def u_prefetch(upto_i):
        while ust["next"] <= upto_i and ust["next"] < NTILE_C * 144:
            i = ust["next"]
            ust["next"] += 1
            r = i % 144
            src = w_q[r] if r < 16 else UT[r - 16]
            sl_ = i % NUS
            S.dma(us[sl_][:], src.rearrange("p (k n) -> p k n", n=128), RUS[sl_].key, writes=[RUS[sl_]])

    def u_use():
        i = ust["use"]
        ust["use"] += 1
        u_prefetch(i + NUS - 1)
        return i % NUS

    def v_prefetch(upto_j):
        while vst["next"] <= upto_j and vst["next"] < NTILE_C * 16 * 32:
            j = vst["next"]
            vst["next"] += 1
            sbk_, r_ = (j // 32) % 16, j % 32
            sl_ = j % NVS
            S.dma(vs_[sl_][:], EV[sbk_, r_ // 4, r_ % 4].rearrange("p (c n) -> p c n", n=512), RVS[sl_].key, writes=[RVS[sl_]])

    def v_use():
        j = vst["use"]
        vst["use"] += 1
        v_prefetch(j + NVS - 1)
        return j % NVS
    zbuf = sb("zbuf", [128, 4096], F32)
    qTs = zbuf[:].rearrange("p (g t) -> p g t", g=16)
    R_qT = Res("qTs")
    GAT = sb("GAT", [128, 8, TC], F32R)
    R_GAT = Res("GAT")
    s12 = [sb("s12%d" % i, [128, 16, 128], F32) for i in range(2)]
    R_s12 = [Res("s12%d" % i) for i in range(2)]
    Gacc = [sb("Gacc%d" % i, [128, 8, 128], F32) for i in range(2)]
    R_G = [Res("Gacc%d" % i) for i in range(2)]
    zt = [zbuf[:, i * 1024:(i + 1) * 1024].rearrange("p (c n) -> p c n", n=128) for i in range(2)]
    ezt = [zbuf[:, (2 + i) * 1024:(3 + i) * 1024].rearrange("p (c n) -> p c n", n=128) for i in range(2)]
    R_z = [Res("zt%d" % i) for i in range(2)]
    R_ez = [Res("ezt%d" % i) for i in range(2)]
    AgT = [sb("AgT%d" % i, [128, TC], F32) for i in range(2)]
    R_Ag = [Res("AgT%d" % i) for i in range(2)]
    keys_sb = sb("keys_sb", [128, 16, 128], F32)
    R_keys = Res("keys")
    S.dma(keys_sb[:], keysT, "keys", writes=[R_keys])
    sq2 = [sb("sqc%d" % i, [128, TC], F32R) for i in range(2)]
    RSQ2 = [Res("sqc%d" % i) for i in range(2)]
    rstd2 = sb("rstd2", [128, TC], F32)
    R_rstd2 = Res("rstd2")
    ntmp = [sb("ntmp%d" % i, [128, TC], F32) for i in range(2)]
    RNT = [Res("ntmp%d" % i) for i in range(2)]
    vtop = [sb("vtop%d" % i, [128, 16, 16], F32) for i in range(2)]
    mrt = sb("mrt", [128, 256], F32)
    cand = sb("cand", [128, 8, 16, 16], F32)
    tsv = [sb("tsv%d" % i, [128, 8, 16], F32) for i in range(2)]
    sm2 = [sb("sm2%d" % i, [128, 4, 8], F32) for i in range(2)]
    extmp = sb("extmp", [128, 8, 16], F32)
    R_top = [Res("top%d" % i) for i in range(2)]
    R_mrt, R_cand, R_ext = Res("mrt"), Res("cand"), Res("extmp")
    stg3 = [sb("stgC%d" % i, [128, TC], F32) for i in range(2)]
    RST3 = [Res("stgC%d" % i) for i in range(2)]
    x1Tv = x1T.rearrange("(k p) t -> p k t", p=128)
    ui = 0
    vi = 0
    zi = 0
    ai = 0
    for ti in range(NTOK // TC):
        t0 = ti * TC
        for kq in range(4):
            S.dma(acc[:, kq * 8:(kq + 1) * 8, :], x1Tv[:, kq * 8:(kq + 1) * 8, t0:t0 + TC], "accl%d" % kq,
                  writes=RACC[kq * 8:(kq + 1) * 8])
        for k in range(32):
            q = k % 2
            S.act(sq2[q][:], acc[:, k, :], AF.Square, reads=[RACC[k]], writes=[RSQ2[q]])
            S.mm(ps[0][:, 0:TC], ones[:], sq2[q][:], k == 0, k == 31, reads=[RSQ2[q], R_const], writes=[RP[0]])
        S.ts(rstd2[:], ps[0][:, 0:TC], 1.0 / D, EPS, ALU.mult, ALU.add, reads=[RP[0]], writes=[R_rstd2])
        S.act(rstd2[:], rstd2[:], AF.Sqrt, reads=[R_rstd2], writes=[R_rstd2])
        S.recip(rstd2[:], rstd2[:], reads=[R_rstd2], writes=[R_rstd2])
        for k in range(32):
            q = k % 2
            S.tt(ntmp[q][:], acc[:, k, :], rstd2[:], ALU.mult, reads=[RACC[k], R_rstd2], writes=[RNT[q]], eng="pool")
            S.ts(h2T[:, k, :], ntmp[q][:], gs2[:, k:k + 1], sh2[:, k:k + 1], ALU.mult, ALU.add,
                 reads=[RNT[q], R_mod], writes=[R_h2])
        for g in range(16):
            sl = u_use()
            bq = 1 + g % 2
            for k in range(32):
                S.mm(ps[bq][:, 0:TC], us[sl][:, k, :], h2T[:, k, :], k == 0, k == 31, reads=[RUS[sl], R_h2], writes=[RP[bq]])
            S.cp(qTs[:, g, :], ps[bq][:, 0:TC], reads=[RP[bq]], writes=[R_qT, R_z[0], R_z[1], R_ez[0], R_ez[1]], eng=("act" if g % 2 else "dve"))
        for u in range(2):
            for gq in range(4):
                bank = 3 + gq % 2
                for gg in range(4):
                    g = gq * 4 + gg
                    S.mm(ps[bank][:, gg * 128:(gg + 1) * 128], qTs[:, g, u * 128:(u + 1) * 128], keys_sb[:, g, :], True, True,
                         reads=[R_qT, R_z[0], R_z[1], R_ez[0], R_ez[1], R_keys], writes=[RP[bank]])
                S.cp(s12[u][:, gq * 4:(gq + 1) * 4, :], ps[bank][:, 0:512].rearrange("p (a b) -> p a b", a=4),
                     reads=[RP[bank]], writes=[R_s12[u]], eng="act")
            for g in range(16):
                S.max8(vtop[u][:, g, 0:8], s12[u][:, g, :], reads=[R_s12[u]], writes=[R_top[u]])
                S.mrep(mrt[:, 0:128], vtop[u][:, g, 0:8], s12[u][:, g, :], reads=[R_s12[u], R_top[u]], writes=[R_mrt])
                S.max8(vtop[u][:, g, 8:16], mrt[:, 0:128], reads=[R_mrt], writes=[R_top[u]])
            v1 = vtop[u][:, 0:16:2, :].unsqueeze(3).to_broadcast([128, 8, 16, 16])
            v2 = vtop[u][:, 1:16:2, :].unsqueeze(2).to_broadcast([128, 8, 16, 16])
            S.tt(cand[:], v1, v2, ALU.add, reads=[R_top[u]], writes=[R_cand])
            for hh in range(8):
                cflat = cand[:, hh, :, :].rearrange("p a b -> p (a b)")
                S.max8(tsv[u][:, hh, 0:8], cflat, reads=[R_cand], writes=[R_top[u]])
                S.mrep(mrt[:], tsv[u][:, hh, 0:8], cflat, reads=[R_cand, R_top[u]], writes=[R_mrt])
                S.max8(tsv[u][:, hh, 8:16], mrt[:], reads=[R_mrt], writes=[R_top[u]])
            S.ts(sm2[u][:, 0, :], tsv[u][:, :, 0], -1.0, None, ALU.mult, reads=[R_top[u]], writes=[R_top[u]])
            S.cp(sm2[u][:, 1, :], tsv[u][:, :, 15], reads=[R_top[u]], writes=[R_top[u]])
            S.tt(extmp[:], tsv[u][:], sm2[u][:, 0, :].unsqueeze(2).to_broadcast([128, 8, 16]), ALU.add,
                 reads=[R_top[u]], writes=[R_ext])
            S.act(extmp[:], extmp[:], AF.Exp, reads=[R_ext], writes=[R_ext])
            S.rsum(sm2[u][:, 2, :], extmp[:], reads=[R_ext], writes=[R_top[u]])
            S.recip(sm2[u][:, 3, :], sm2[u][:, 2, :], reads=[R_top[u]], writes=[R_top[u]])
        def G_iter(sbk_, u, hh):
            nonlocal zi
            cb_ = sbk_ * 8
            zb = zi % 2
            zi += 1
            s2b = s12[u][:, 2 * hh + 1, :].unsqueeze(1).to_broadcast([128, 8, 128])
            s1b = s12[u][:, 2 * hh, cb_:cb_ + 8].unsqueeze(2).to_broadcast([128, 8, 128])
            S.tt(zt[zb][:], s2b, s1b, ALU.add, reads=[R_s12[u]], writes=[R_z[zb]], eng="pool")
            S.act(ezt[zb][:], zt[zb][:], AF.Exp, bias=sm2[u][:, 0, hh:hh + 1], reads=[R_z[zb], R_top[u]], writes=[R_ez[zb]])
            S.stt(ezt[zb][:], zt[zb][:], sm2[u][:, 1, hh:hh + 1], ezt[zb][:], ALU.is_ge, ALU.mult,
                  reads=[R_z[zb], R_ez[zb], R_top[u]], writes=[R_ez[zb]])
            if hh == 0:
                S.ts(Gacc[u][:], ezt[zb][:], sm2[u][:, 3, hh:hh + 1], None, ALU.mult,
                     reads=[R_ez[zb], R_top[u]], writes=[R_G[u]])
            else:
                S.stt(Gacc[u][:], ezt[zb][:], sm2[u][:, 3, hh:hh + 1], Gacc[u][:], ALU.mult, ALU.add,
                      reads=[R_ez[zb], R_top[u], R_G[u]], writes=[R_G[u]])

        for u in range(2):
            for hh in range(8):
                G_iter(0, u, hh)
        for sbk in range(NSB):
            v_prefetch(((ti * NSB) + sbk) * 32 + NVS - 1)
            for cc in range(8):
                sl = u_use()
                ba = 1 + ai % 2
                aq = ai % 2
                ai += 1
                for k in range(32):
                    S.mm(ps[ba][:, 0:TC], us[sl][:, k, :], h2T[:, k, :], k == 0, k == 31, reads=[RUS[sl], R_h2], writes=[RP[ba]])
                S.act(AgT[aq][:], ps[ba][:, 0:TC], AF.Gelu_apprx_tanh, reads=[RP[ba]], writes=[R_Ag[aq]])
                for u in range(2):
                    S.tr(ps[5][:, u * 128:(u + 1) * 128], Gacc[u][:, cc, :], ident[:], reads=[R_G[u], R_const], writes=[RP[5]])
                S.tt(GAT[:, cc, :], AgT[aq][:], ps[5][:, 0:TC], ALU.mult, reads=[R_Ag[aq], RP[5]], writes=[R_GAT])
            giters = [(u, hh) for u in range(2) for hh in range(8)] if sbk + 1 < NSB else []
            for dg in range(8):
                banks = (6, 7) if dg % 2 == 0 else (3, 4)
                for ccp in range(4):
                    sl = v_use()
                    for c2 in range(2):
                        cc = ccp * 2 + c2
                        for sub in range(2):
                            S.mm(ps[banks[sub]][:], GAT[:, cc, sub * 128:(sub + 1) * 128], vs_[sl][:, c2, :], cc == 0, cc == 7,
                                 reads=[R_GAT, RVS[sl]], writes=[RP[banks[sub]]])
                for sub in range(2):
                    rr = [RACC[sub * 16 + 2 * dg], RACC[sub * 16 + 2 * dg + 1]]
                    dst = P_tok[sub][:, dg * 512:(dg + 1) * 512]
                    if sbk == 0:
                        S.cp(dst, ps[banks[sub]][:], reads=[RP[banks[sub]]], writes=rr)
                    else:
                        S.tt(dst, ps[banks[sub]][:], dst, ALU.add, reads=[RP[banks[sub]]] + rr, writes=rr)
                for _ in range(2):
                    if giters:
                        G_iter(sbk + 1, *giters.pop(0))
        for m in range(32):
            q = m % 2
            S.dma(xmc[q][:], x1T[m * 128:(m + 1) * 128, t0:t0 + TC], RXMC[q].key, writes=[RXMC[q]], q="act")
            rr = [RACC[m // 2], RACC[16 + m // 2]]
            for sub in range(2):
                S.tr(ps[5][:, sub * 128:(sub + 1) * 128], P_tok[sub][:, m * 128:(m + 1) * 128], ident[:], reads=[rr[sub], R_const], writes=[RP[5]])
            x2v = acc2[:, :, m * 128:(m + 1) * 128]
            S.stt(x2v, ps[5][:, 0:TC].rearrange("p (s t) -> p s t", s=2), gate2[:, m:m + 1],
                  xmc[q][:].rearrange("p (s t) -> p s t", s=2), ALU.mult, ALU.add,
                  reads=[RP[5], RXMC[q], R_mod] + rr, writes=rr)
            S.act(sq2[q][:].rearrange("p (s t) -> p s t", s=2), x2v, AF.Square, reads=rr, writes=[RSQ2[q]])
            S.mm(ps[0][:, 0:TC], ones[:], sq2[q][:], m == 0, m == 31, reads=[RSQ2[q], R_const], writes=[RP[0]])
        S.ts(rstd2[:], ps[0][:, 0:TC], 1.0 / D, EPS, ALU.mult, ALU.add, reads=[RP[0]], writes=[R_rstd2])
        S.act(rstd2[:], rstd2[:], AF.Sqrt, reads=[R_rstd2], writes=[R_rstd2])
        S.recip(rstd2[:], rstd2[:], reads=[R_rstd2], writes=[R_rstd2])
        for m in range(32):
            q = m % 2
            rr = [RACC[m // 2], RACC[16 + m // 2]]
            x2v = acc2[:, :, m * 128:(m + 1) * 128]
            S.tt(ntmp[q][:].rearrange("p (s t) -> p s t", s=2), x2v, rstd2[:].rearrange("p (s t) -> p s t", s=2), ALU.mult,
                 reads=rr + [R_rstd2], writes=[RNT[q]], eng="pool")
            S.ts(stg3[q][:], ntmp[q][:], gfs[:, m:m + 1], None, ALU.mult, reads=[RNT[q], R_const], writes=[RST3[q]])
            S.dma(outT[m * 128:(m + 1) * 128, t0:t0 + TC], stg3[q][:], RST3[q].key, reads=[RST3[q]], q="act")
    S.barrier()
    esD.close()
    S.emit()
    return nc


def _bias_tables(rpb_l, top_edge, bot_edge):
    H = 16
    qc = np.arange(64)
    cstart = np.clip(qc - 8, 0, 48)

    def table(width_rows, row_off, qrows_abs, rs_abs):
        W = width_rows * 64
        tab = np.full((H, 128, W), NEG, np.float32)
        for qr in range(2):
            rs = rs_abs[qr]
            for i in range(8):
                krow = rs + i
                wr = krow - row_off
                if wr < 0 or wr >= width_rows:
                    continue
                dr = krow - qr + 7
                for x in range(64):
                    ys = cstart[x] + np.arange(16)
                    dc = ys - x + 15
                    tab[:, qr * 64 + x, wr * 64 + ys] = rpb_l[:, dr, dc]
        return tab

    gen = table(10, -4, None, (-4, -3))
    if top_edge:
        top = table(12, -4, None, (0, 0))
    else:
        top = table(12, -4, None, (-4, -3))
    if bot_edge:
        bot = table(12, -6, None, (-6, -6))
    else:
        bot = table(12, -6, None, (-4, -3))
    top1 = table(10, -4, None, (-2, -2)) if top_edge else gen
    bot14 = table(10, -4, None, (-4, -4)) if bot_edge else gen
    return gen, top, bot, top1, bot14


_NC_CACHE = {}


def _prep_inputs(inp):
    x = np.asarray(inp["x"], np.float32)
    c = np.asarray(inp["c"], np.float32)
    l = 0
    shared = {
        "w_ada": np.ascontiguousarray(inp["w_ada"][l], np.float32),
        "b_ada": np.ascontiguousarray(inp["b_ada"][l].reshape(1, -1), np.float32),
        "g1": np.ascontiguousarray(inp["norm1_g"][l].reshape(32, 128).T, np.float32),
        "g2": np.ascontiguousarray(inp["norm2_g"][l].reshape(32, 128).T, np.float32),
        "gf": np.ascontiguousarray(np.asarray(inp["norm_f_g"]).reshape(32, 128).T, np.float32),
        "w_in": np.ascontiguousarray(np.asarray(inp["w_in"][l], np.float32).reshape(32, 128, 80, 256).transpose(2, 1, 0, 3)).reshape(80, 128, 8192),
        "convw": np.ascontiguousarray(np.asarray(inp["conv_w"][l]).reshape(3, 16, 128).transpose(2, 1, 0), np.float32),
        "w_ao": np.ascontiguousarray(np.asarray(inp["w_attn_out"][l], np.float32).reshape(16, 128, 16, 256).transpose(2, 1, 0, 3)).reshape(16, 128, 4096),
        "w_co": np.ascontiguousarray(np.asarray(inp["w_conv_out"][l], np.float32).reshape(16, 128, 16, 256).transpose(2, 1, 0, 3)).reshape(16, 128, 4096),
        "w_o": np.ascontiguousarray(np.asarray(inp["w_o"][l], np.float32).reshape(32, 128, 16, 256).transpose(2, 1, 0, 3)).reshape(16, 128, 8192),
        "w_q": np.ascontiguousarray(np.asarray(inp["w_q_peer"][l], np.float32).reshape(32, 128, 16, 128).transpose(2, 1, 0, 3)).reshape(16, 128, 4096),
        "UT": np.ascontiguousarray(np.asarray(inp["expert_u"][l], np.float32).reshape(128, 128, 32, 128).transpose(0, 3, 2, 1)).reshape(128, 128, 4096),
        "EV": np.ascontiguousarray(np.asarray(inp["expert_v"][l], np.float32).reshape(16, 4, 2, 128, 8, 512).transpose(0, 4, 1, 3, 2, 5)).reshape(16, 8, 4, 128, 1024),
        "ident": np.eye(128, dtype=np.float32),
    }
    k1 = np.asarray(inp["sub_keys_1"][l])
    k2 = np.asarray(inp["sub_keys_2"][l])
    kk = np.stack([k1, k2], axis=1).reshape(16, 128, 128)
    shared["keysT"] = np.ascontiguousarray(kk.transpose(2, 0, 1), np.float32)
    rpb_l = np.asarray(inp["rpb"][l], np.float32)
    tabs = {}
    maps = []
    for core in range(8):
        b, qd = core // 4, core % 4
        t0 = qd * NTOK
        xe = np.zeros((NEXT, D), np.float32)
        lo, hi = t0 - HALO, t0 + NTOK + HALO
        slo, shi = max(lo, 0), min(hi, 8192)
        xe[slo - lo: shi - lo] = x[b, slo:shi]
        m = dict(shared)
        m["xT"] = np.ascontiguousarray(xe.T)
        m["cT"] = np.ascontiguousarray(c[b].reshape(32, 128).T, np.float32)
        te, be = (qd == 0), (qd == 3)
        if (te, be) not in tabs:
            tabs[(te, be)] = _bias_tables(rpb_l, te, be)
        g_, t_, b_, t1_, b14_ = tabs[(te, be)]
        m["biasg"], m["biast"], m["biasb"], m["bias1"], m["bias14"] = g_, t_, b_, t1_, b14_
        fl = np.ones((128, 2), np.float32)
        if te:
            fl[:, 0] = 0.0
        if be:
            fl[:, 1] = 0.0
        m["flags"] = fl
        maps.append(m)
    return maps


def kernel(**inputs):
    maps = _prep_inputs(inputs)
    if "nc" not in _NC_CACHE:
        _NC_CACHE["nc"] = build_nc()
    nc = _NC_CACHE["nc"]
    res = run_bass_kernel_spmd(nc, maps, core_ids=list(range(8)))
    out = np.zeros((2, 8192, D), np.float32)
    for core in range(8):
        b, qd = core // 4, core % 4
        out[b, qd * NTOK:(qd + 1) * NTOK] = res.results[core]["outT"].T
    return out
```

```python
import numpy as np
import concourse.bass as bass
import concourse.mybir as mybir
from concourse.bass_utils import run_bass_kernel_spmd
from contextlib import ExitStack

F32 = mybir.dt.float32
F32R = mybir.dt.float32r
AF = mybir.ActivationFunctionType
ALU = mybir.AluOpType
AX = mybir.AxisListType

D = 4096
NTOK = 2048
HALO = 256
NEXT = NTOK + 2 * HALO
INC = 20480
NEG = -1.0e30
EPS = 1e-6
SCALE = 128 ** -0.5


class Res:
    __slots__ = ("name", "w", "r", "key")

    def __init__(self, name, key=None):
        self.name = name
        self.w = {}
        self.r = {}
        self.key = key if key is not None else name


class Op:
    __slots__ = ("eng", "fn", "deps", "need", "val", "dma_key", "idx")


class Sched:
    ENG = ("pe", "act", "dve", "pool", "sp")

    def __init__(self, nc):
        self.nc = nc
        self.ops = {e: [] for e in self.ENG}
        self.dma_cnt = {}
        self.n = 0
        self.strict = True

    def op(self, eng, fn, reads=(), writes=(), dma_key=None):
        o = Op()
        o.eng = eng
        o.fn = fn
        o.need = False
        o.val = None
        o.dma_key = dma_key
        o.idx = self.n
        self.n += 1
        deps = {}
        for r in reads:
            for d in r.w.values():
                deps[id(d)] = d
        for w in writes:
            for d in w.w.values():
                deps[id(d)] = d
            for d in w.r.values():
                deps[id(d)] = d
        dl = []
        for d in deps.values():
            if d.dma_key is None and d.eng == eng and dma_key is None and (eng == "pe" or not self.strict):
                continue
            if dma_key is not None and d.dma_key == dma_key:
                continue
            d.need = True
            dl.append(d)
        o.deps = dl
        if dma_key is not None:
            c = self.dma_cnt.get(dma_key, 0) + 1
            self.dma_cnt[dma_key] = c
            o.val = 16 * c
            key = "dma:" + dma_key
        else:
            key = eng
        for r in reads:
            r.r[key] = o
        for w in writes:
            w.w = {key: o}
            w.r = {}
        self.ops[eng].append(o)
        return o

    def barrier(self):
        lasts = []
        for e in self.ENG:
            for o_ in reversed(self.ops[e]):
                if o_.fn is not None and o_.dma_key is None:
                    lasts.append(o_)
                    break
        dmas = dict(self.dma_cnt)
        for e in self.ENG:
            o = Op()
            o.eng = e
            o.fn = None
            o.need = False
            o.val = None
            o.dma_key = None
            o.idx = self.n
            self.n += 1
            dl = []
            for l in lasts:
                if l.eng != e and l.dma_key is None:
                    l.need = True
                    dl.append(l)
            o.deps = dl + [("dma", k, 16 * c) for k, c in dmas.items()]
            self.ops[e].append(o)

    def emit(self):
        nc = self.nc
        for e in self.ENG:
            c = 0
            for o in self.ops[e]:
                if o.dma_key is None and o.need:
                    c += 1
                    o.val = c
        sems = {}

        def sem(k):
            if k not in sems:
                sems[k] = nc.alloc_semaphore(name="s_" + k.replace(":", "_"))
            return sems[k]

        handles = {"pe": nc.tensor, "act": nc.scalar, "dve": nc.vector, "pool": nc.gpsimd, "sp": nc.sync}

        def run(e, h):
            waited = {}
            for o in self.ops[e]:
                for d in o.deps:
                    if isinstance(d, tuple):
                        k, v = "dma:" + d[1], d[2]
                    elif d.dma_key is not None:
                        k, v = "dma:" + d.dma_key, d.val
                    else:
                        k, v = d.eng, d.val
                    if waited.get(k, 0) < v:
                        h.wait_ge(sem(k), v)
                        waited[k] = v
                if o.fn is None:
                    continue
                ins = o.fn(h)
                if o.dma_key is not None:
                    ins.then_inc(sem("dma:" + o.dma_key), 16)
                elif o.need:
                    ins.then_inc(sem(e), 1)

        with nc.Block() as block:
            @block.tensor
            def _(h):
                run("pe", h)

            @block.scalar
            def _(h):
                run("act", h)

            @block.vector
            def _(h):
                run("dve", h)

            @block.gpsimd
            def _(h):
                run("pool", h)

            @block.sync
            def _(h):
                run("sp", h)

    def dma(self, out, in_, key, reads=(), writes=(), q="sp"):
        return self.op(q, lambda h: h.dma_start(out=out, in_=in_), reads, writes, dma_key=key)

    def mm(self, out, lhsT, rhs, start, stop, reads=(), writes=()):
        return self.op("pe", lambda h: h.matmul(out, lhsT, rhs, start=start, stop=stop, skip_group_check=True), reads, writes)

    def tr(self, out, in_, ident, reads=(), writes=()):
        return self.op("pe", lambda h: h.transpose(out, in_, ident), reads, writes)

    def act(self, out, in_, func, reads=(), writes=(), eng="act", **kw):
        return self.op(eng, lambda h: h.activation(out=out, in_=in_, func=func, **kw), reads, writes)

    def tt(self, out, in0, in1, op, reads=(), writes=(), eng="dve"):
        return self.op(eng, lambda h: h.tensor_tensor(out=out, in0=in0, in1=in1, op=op), reads, writes)

    def ts(self, out, in0, s1, s2, op0, op1=None, reads=(), writes=(), eng="dve"):
        if op1 is None:
            return self.op(eng, lambda h: h.tensor_scalar(out=out, in0=in0, scalar1=s1, scalar2=None, op0=op0), reads, writes)
        return self.op(eng, lambda h: h.tensor_scalar(out=out, in0=in0, scalar1=s1, scalar2=s2, op0=op0, op1=op1), reads, writes)

    def stt(self, out, in0, scalar, in1, op0, op1, reads=(), writes=()):
        return self.op("dve", lambda h: h.scalar_tensor_tensor(out=out, in0=in0, scalar=scalar, in1=in1, op0=op0, op1=op1), reads, writes)

    def cp(self, out, in_, reads=(), writes=(), eng="dve"):
        if eng == "act":
            return self.op("act", lambda h: h.copy(out=out, in_=in_), reads, writes)
        return self.op(eng, lambda h: h.tensor_copy(out=out, in_=in_), reads, writes)

    def rmax(self, out, in_, reads=(), writes=()):
        return self.op("dve", lambda h: h.reduce_max(out=out, in_=in_, axis=AX.X), reads, writes)

    def rsum(self, out, in_, reads=(), writes=()):
        return self.op("dve", lambda h: h.reduce_sum(out=out, in_=in_, axis=AX.X), reads, writes)

    def recip(self, out, in_, reads=(), writes=()):
        return self.op("dve", lambda h: h.reciprocal(out=out, in_=in_), reads, writes)

    def max8(self, out, in_, reads=(), writes=()):
        return self.op("dve", lambda h: h.max(out=out, in_=in_), reads, writes)

    def mrep(self, out, rep, vals, reads=(), writes=()):
        return self.op("dve", lambda h: h.match_replace(out=out, in_to_replace=rep, in_values=vals, imm_value=NEG), reads, writes)

    def gen(self, eng, fn, reads=(), writes=()):
        return self.op(eng, fn, reads, writes)


def build_nc(upto=99, debug=False):
    nc = bass.Bass("TRN2", target_bir_lowering=False)
    nc.dge_precook = False
    S = Sched(nc)
    skind = "ExternalOutput" if debug else "Internal"

    def din(name, shape, dt=F32):
        return nc.dram_tensor(name, list(shape), dt, kind="ExternalInput").ap()

    xT = din("xT", [D, NEXT])
    cT = din("cT", [128, 32], F32R)
    w_ada = din("w_ada", [D, 6 * D], F32R)
    b_ada = din("b_ada", [1, 6 * D])
    g1 = din("g1", [128, 32])
    g2 = din("g2", [128, 32])
    gf = din("gf", [128, 32])
    w_in = din("w_in", [80, 128, 32 * 256], F32R)
    biasg = din("biasg", [16, 128, 640])
    bias1 = din("bias1", [16, 128, 640])
    bias14 = din("bias14", [16, 128, 640])
    biast = din("biast", [16, 128, 768])
    biasb = din("biasb", [16, 128, 768])
    convw = din("convw", [128, 16, 3])
    w_ao = din("w_ao", [16, 128, 16 * 256], F32R)
    w_co = din("w_co", [16, 128, 16 * 256], F32R)
    w_o = din("w_o", [16, 128, 32 * 256], F32R)
    w_q = din("w_q", [16, 128, 32 * 128], F32R)
    keysT = din("keysT", [128, 16, 128])
    UT = din("UT", [128, 128, 32 * 128], F32R)
    EV = din("EV", [16, 8, 4, 128, 2 * 512], F32R)
    ident_d = din("ident", [128, 128])
    flags_d = din("flags", [128, 2])
    outT = nc.dram_tensor("outT", [D, NTOK], F32, kind="ExternalOutput").ap()
    featT = nc.dram_tensor("featT", [INC, NEXT], F32R, kind=skind).ap()
    vtok = nc.dram_tensor("vtok", [NEXT, 2048], F32R, kind=skind).ap()
    attnT = nc.dram_tensor("attnT", [2048, NTOK], F32R, kind=skind).ap()
    x1T = nc.dram_tensor("x1T", [D, NTOK], F32, kind=skind).ap()
    modd = nc.dram_tensor("modd", [128, 6 * 32], F32, kind=skind).ap()

    sb = nc.alloc_sbuf_tensor
    mod = sb("mod", [128, 6, 32], F32)
    gs1 = sb("gs1", [128, 32], F32)
    gs2 = sb("gs2", [128, 32], F32)
    g1s = sb("g1s", [128, 32], F32)
    g2s = sb("g2s", [128, 32], F32)
    gfs = sb("gfs", [128, 32], F32)
    ones = sb("ones", [128, 128], F32R)
    ident = sb("identsb", [128, 128], F32)
    flags = sb("flagssb", [128, 2], F32)
    one1 = sb("one1", [1, 1], F32)
    R_const = Res("const")
    ps = [nc.alloc_psum_tensor("ps%d" % i, [128, 512], F32) for i in range(8)]
    RP = [Res("ps%d" % i) for i in range(8)]

    S.dma(g1s[:], g1, "const", writes=[R_const])
    S.dma(g2s[:], g2, "const", writes=[R_const])
    S.dma(gfs[:], gf, "const", writes=[R_const])
    S.dma(ident[:], ident_d, "const", writes=[R_const])
    S.dma(flags[:], flags_d, "const", writes=[R_const])
    onesf = sb("onesf", [128, 128], F32)
    S.gen("dve", lambda h: h.memset(onesf[:], 1.0), writes=[R_const])
    S.cp(ones[:], onesf[:], reads=[R_const], writes=[R_const])
    S.gen("dve", lambda h: h.memset(one1[:], 1.0), writes=[R_const])

    es0 = ExitStack()
    sb0 = lambda n, sh, dt: es0.enter_context(nc.sbuf_tensor(n, sh, dt))
    cTs = sb0("cTs", [128, 32], F32R)
    was = [sb0("wa%d" % i, [128, 4096], F32R) for i in range(3)]
    RWA = [Res("wa%d" % i) for i in range(3)]
    rowb = sb0("rowb", [1, 4096], F32)
    row = sb0("row", [1, 4096], F32)
    R_cT, R_rowb, R_row, R_mod = Res("cT"), Res("rowb"), Res("row"), Res("mod")
    S.dma(cTs[:], cT, "cT", writes=[R_cT])
    li = 0
    for g in range(6):
        S.dma(rowb[:], b_ada[:, g * 4096:(g + 1) * 4096], "rowb", writes=[R_rowb])
        for k in range(32):
            sl = li % 3
            li += 1
            for hh in range(2):
                S.dma(was[sl][:, hh * 2048:(hh + 1) * 2048],
                      w_ada[k * 128:(k + 1) * 128, g * 4096 + hh * 2048: g * 4096 + (hh + 1) * 2048],
                      "wa%d" % sl, writes=[RWA[sl]])
            for b in range(8):
                S.mm(ps[b][0:1, :], cTs[:, k:k + 1], was[sl][:, b * 512:(b + 1) * 512], k == 0, k == 31,
                     reads=[R_cT, RWA[sl]], writes=[RP[b]])
        for b in range(8):
            S.tt(row[:, b * 512:(b + 1) * 512], ps[b][0:1, :], rowb[:, b * 512:(b + 1) * 512], ALU.add,
                 reads=[RP[b], R_rowb], writes=[R_row])
        for j in range(32):
            S.mm(ps[0][:, j:j + 1], row[0:1, j * 128:(j + 1) * 128], one1[0:1, 0:1], True, True,
                 reads=[R_row, R_const], writes=[RP[0]])
        S.cp(mod[:, g, :], ps[0][:, 0:32], reads=[RP[0]], writes=[R_mod])
    S.stt(gs1[:], mod[:, 1, :], 1.0, g1s[:], ALU.add, ALU.mult, reads=[R_mod, R_const], writes=[R_mod])
    S.stt(gs2[:], mod[:, 4, :], 1.0, g2s[:], ALU.add, ALU.mult, reads=[R_mod, R_const], writes=[R_mod])
    if debug:
        S.dma(modd, mod[:].rearrange("p a b -> p (a b)"), "modd", reads=[R_mod])
    sh1, gate1, sh2, gate2 = mod[:, 0, :], mod[:, 2, :], mod[:, 3, :], mod[:, 5, :]
    S.barrier()
    es0.close()
    if upto < 1:
        S.emit()
        return nc

    def norm_tile(src_load, T, dst, gs, sh, nk=32):
        for kq in range(8):
            sl = kq % 2
            src_load(kq, xs[sl], RXS[sl])
            for kk in range(4):
                k = kq * 4 + kk
                q = k % 2
                S.act(sq[q][:, 0:T], xs[sl][:, kk, 0:T], AF.Square, reads=[RXS[sl]], writes=[RSQ[q]])
                S.mm(ps[0][:, 0:T], ones[:], sq[q][:, 0:T], k == 0, k == 31, reads=[RSQ[q], R_const], writes=[RP[0]])
        S.ts(rstd[:, 0:T], ps[0][:, 0:T], 1.0 / D, EPS, ALU.mult, ALU.add, reads=[RP[0]], writes=[R_rstd])
        S.act(rstd[:, 0:T], rstd[:, 0:T], AF.Sqrt, reads=[R_rstd], writes=[R_rstd])
        S.recip(rstd[:, 0:T], rstd[:, 0:T], reads=[R_rstd], writes=[R_rstd])
        for kq in range(8):
            sl = kq % 2
            src_load(kq, xs[sl], RXS[sl])
            for kk in range(4):
                k = kq * 4 + kk
                S.tt(xs[sl][:, kk, 0:T], xs[sl][:, kk, 0:T], rstd[:, 0:T], ALU.mult, reads=[RXS[sl], R_rstd], writes=[RXS[sl]], eng="pool")
                S.ts(dst[:, k, 0:T], xs[sl][:, kk, 0:T], gs[:, k:k + 1], sh[:, k:k + 1], ALU.mult, ALU.add,
                     reads=[RXS[sl], R_mod], writes=[R_dst[0]])

    esA = ExitStack()
    sb = lambda n, sh, dt: esA.enter_context(nc.sbuf_tensor(n, sh, dt))
    xs = [sb("xs%d" % i, [128, 4, 512], F32) for i in range(2)]
    RXS = [Res("xs%d" % i) for i in range(2)]
    sq = [sb("sq%d" % i, [128, 512], F32R) for i in range(2)]
    RSQ = [Res("sq%d" % i) for i in range(2)]
    rstd = sb("rstd", [128, 512], F32)
    R_rstd = Res("rstd")
    hT = sb("hT", [128, 32, 512], F32R)
    R_hT = Res("hT")
    R_dst = [R_hT]
    ws = [sb("ws%d" % i, [128, 32, 256], F32R) for i in range(3)]
    RWS = [Res("ws%d" % i) for i in range(3)]
    stg = [sb("stg%d" % i, [128, 512], F32R) for i in range(4)]
    RSTG = [Res("stgA%d" % i) for i in range(4)]
    xTv = xT.rearrange("(k p) t -> p k t", p=128)
    vtokv = vtok
    wi = 0
    si = 0
    pi = 0
    for ti in range(5):
        if ti < 4:
            segs = [(HALO + 512 * ti, 0, 512)]
        else:
            segs = [(0, 0, 256), (NTOK + HALO, 256, 256)]

        def src_load(kq, slot, res, segs=segs):
            for (e0, o0, ln) in segs:
                S.dma(slot[:, :, o0:o0 + ln], xTv[:, kq * 4:(kq + 1) * 4, e0:e0 + ln], res.key, writes=[res])
        norm_tile(src_load, 512, hT, gs1, sh1)
        if ti < 4:
            cslots = list(range(80))
        else:
            cslots = list(range(8, 24)) + list(range(32, 48))
        for cs in cslots:
            c0 = cs * 256
            sl = wi % 3
            wi += 1
            for kq in range(2):
                S.dma(ws[sl][:, kq * 16:(kq + 1) * 16, :],
                      w_in[cs][:, kq * 4096:(kq + 1) * 4096].rearrange("p (k n) -> p k n", n=256), "ws%d" % sl, writes=[RWS[sl]])
            isv = 4096 <= c0 < 6144
            if isv:
                for sub in range(4):
                    b = 1 + pi % 4
                    pi += 1
                    for k in range(32):
                        S.mm(ps[b][:, 0:256], hT[:, k, sub * 128:(sub + 1) * 128], ws[sl][:, k, :], k == 0, k == 31,
                             reads=[R_hT, RWS[sl]], writes=[RP[b]])
                    st = si % 4
                    si += 1
                    S.cp(stg[st][:, 0:256], ps[b][:, 0:256], reads=[RP[b]], writes=[RSTG[st]], eng="act")
                    o = sub * 128
                    for (e0, o0, ln) in segs:
                        if o0 <= o < o0 + ln:
                            S.dma(vtokv[e0 + o - o0: e0 + o - o0 + 128, c0 - 4096: c0 - 4096 + 256], stg[st][:, 0:256],
                                  RSTG[st].key, reads=[RSTG[st]], q="act")
            else:
                for half in range(2):
                    b = 1 + pi % 4
                    pi += 1
                    for k in range(32):
                        S.mm(ps[b][:], ws[sl][:, k, half * 128:(half + 1) * 128], hT[:, k, :], k == 0, k == 31,
                             reads=[R_hT, RWS[sl]], writes=[RP[b]])
                    st = si % 4
                    si += 1
                    S.cp(stg[st][:], ps[b][:], reads=[RP[b]], writes=[RSTG[st]], eng="act")
                    r0 = c0 + half * 128
                    for (e0, o0, ln) in segs:
                        S.dma(featT[r0:r0 + 128, e0:e0 + ln], stg[st][:, o0:o0 + ln], RSTG[st].key, reads=[RSTG[st]], q="act")
    S.barrier()
    esA.close()
    if upto < 2:
        S.emit()
        return nc

    esB = ExitStack()
    sb = lambda n, sh, dt: esB.enter_context(nc.sbuf_tensor(n, sh, dt))
    qh = [sb("qh%d" % i, [128, 2048], F32R) for i in range(2)]
    kh = [sb("kh%d" % i, [128, NEXT], F32R) for i in range(2)]
    vh = [sb("vh%d" % i, [128, 20, 128], F32R) for i in range(2)]
    bgs = [sb("bg%d" % i, [128, 640], F32) for i in range(2)]
    b1s = [sb("b1%d" % i, [128, 640], F32) for i in range(2)]
    b14s = [sb("b14%d" % i, [128, 640], F32) for i in range(2)]
    bts = [sb("bt%d" % i, [128, 768], F32) for i in range(2)]
    bbs = [sb("bb%d" % i, [128, 768], F32) for i in range(2)]
    RQ = [Res("qh%d" % i) for i in range(2)]
    RK = [Res("kh%d" % i) for i in range(2)]
    RV = [Res("vh%d" % i) for i in range(2)]
    RB = [Res("bias%d" % i) for i in range(2)]
    sbt = [sb("sbt%d" % i, [128, 768], F32) for i in range(2)]
    Pn = [sb("Pn%d" % i, [128, 768], F32) for i in range(2)]
    PTs = [sb("PTs%d" % i, [128, 768], F32R) for i in range(2)]
    smx = [sb("smx%d" % i, [128, 4], F32) for i in range(2)]
    ah = [sb("ah%d" % i, [128, 2048], F32R) for i in range(2)]
    R_sbt = [Res("sbt%d" % i) for i in range(2)]
    R_Pn = [Res("Pn%d" % i) for i in range(2)]
    R_PT = [Res("PTs%d" % i) for i in range(2)]
    R_sm = [Res("smx%d" % i) for i in range(2)]
    RAH = [Res("ah%d" % i) for i in range(2)]
    vtv = vtok.rearrange("(n p) c -> p n c", p=128)
    it = 0
    for h in range(16):
        p = h % 2
        S.dma(qh[p][:], featT[h * 128:(h + 1) * 128, HALO:HALO + NTOK], RQ[p].key, writes=[RQ[p]])
        S.dma(kh[p][:], featT[2048 + h * 128:2048 + (h + 1) * 128, :], RK[p].key, writes=[RK[p]])
        S.dma(vh[p][:, 0:10, :], vtv[:, 0:10, h * 128:(h + 1) * 128], RV[p].key, writes=[RV[p]])
        S.dma(vh[p][:, 10:20, :], vtv[:, 10:20, h * 128:(h + 1) * 128], RV[p].key, writes=[RV[p]])
        S.dma(bgs[p][:], biasg[h], RB[p].key, writes=[RB[p]])
        S.dma(b1s[p][:], bias1[h], RB[p].key, writes=[RB[p]])
        S.dma(b14s[p][:], bias14[h], RB[p].key, writes=[RB[p]])
        S.dma(bts[p][:], biast[h], RB[p].key, writes=[RB[p]])
        S.dma(bbs[p][:], biasb[h], RB[p].key, writes=[RB[p]])
        for j in range(16):
            if j == 0:
                tiles, bias = list(range(0, 6)), bts[p]
            elif j == 15:
                tiles, bias = list(range(14, 20)), bbs[p]
            elif j == 1:
                tiles, bias = list(range(1, 6)), b1s[p]
            elif j == 14:
                tiles, bias = list(range(14, 19)), b14s[p]
            else:
                tiles, bias = list(range(j, j + 5)), bgs[p]
            nt = len(tiles)
            W = nt * 128
            e0 = tiles[0] * 128
            d = it % 2
            it += 1
            b0, b1_ = 2 + 2 * d, 3 + 2 * d
            qsl = qh[p][:, j * 128:(j + 1) * 128]
            S.mm(ps[b0][:, 0:512], qsl, kh[p][:, e0:e0 + 512], True, True, reads=[RQ[p], RK[p]], writes=[RP[b0]])
            S.mm(ps[b1_][:, 0:W - 512], qsl, kh[p][:, e0 + 512:e0 + W], True, True, reads=[RQ[p], RK[p]], writes=[RP[b1_]])
            S.stt(sbt[d][:, 0:512], ps[b0][:, 0:512], SCALE, bias[:, 0:512], ALU.mult, ALU.add,
                  reads=[RP[b0], RB[p]], writes=[R_sbt[d]])
            S.stt(sbt[d][:, 512:W], ps[b1_][:, 0:W - 512], SCALE, bias[:, 512:W], ALU.mult, ALU.add,
                  reads=[RP[b1_], RB[p]], writes=[R_sbt[d]])
            S.rmax(smx[d][:, 0:1], sbt[d][:, 0:W], reads=[R_sbt[d]], writes=[R_sm[d]])
            S.ts(smx[d][:, 1:2], smx[d][:, 0:1], -1.0, None, ALU.mult, reads=[R_sm[d]], writes=[R_sm[d]])
            S.act(Pn[d][:, 0:W], sbt[d][:, 0:W], AF.Exp, bias=smx[d][:, 1:2], accum_out=smx[d][:, 2:3],
                  reads=[R_sbt[d], R_sm[d]], writes=[R_Pn[d], R_sm[d]])
            S.recip(smx[d][:, 3:4], smx[d][:, 2:3], reads=[R_sm[d]], writes=[R_sm[d]])
            S.ts(Pn[d][:, 0:W], Pn[d][:, 0:W], smx[d][:, 3:4], None, ALU.mult, reads=[R_Pn[d], R_sm[d]], writes=[R_Pn[d]])
            for i in range(nt):
                bank = 6 if i < 4 else 7
                off = (i % 4) * 128
                S.tr(ps[bank][:, off:off + 128], Pn[d][:, i * 128:(i + 1) * 128], ident[:], reads=[R_Pn[d], R_const], writes=[RP[bank]])
            S.cp(PTs[d][:, 0:512], ps[6][:, 0:512], reads=[RP[6]], writes=[R_PT[d]], eng="act")
            S.cp(PTs[d][:, 512:W], ps[7][:, 0:W - 512], reads=[RP[7]], writes=[R_PT[d]], eng="act")
            for i in range(nt):
                S.mm(ps[1][:, 0:128], vh[p][:, tiles[i], :], PTs[d][:, i * 128:(i + 1) * 128], i == 0, i == nt - 1,
                     reads=[RV[p], R_PT[d]], writes=[RP[1]])
            S.cp(ah[p][:, j * 128:(j + 1) * 128], ps[1][:, 0:128], reads=[RP[1]], writes=[RAH[p]], eng="dve")
        S.dma(attnT[h * 128:(h + 1) * 128, :], ah[p][:], RAH[p].key, reads=[RAH[p]], q="act")
    S.barrier()
    esB.close()
    if upto < 3:
        S.emit()
        return nc

    esC = ExitStack()
    sb = lambda n, sh, dt: esC.enter_context(nc.sbuf_tensor(n, sh, dt))
    TB = 256
    aT = sb("aT", [128, 16, TB], F32R)
    cvT = sb("cvT", [128, 16, TB], F32R)
    mg = sb("mg", [128, 32, TB], F32R)
    wsl = [sb("wsl%d" % i, [128, 32, 256], F32R) for i in range(2)]
    RWS2 = [Res("wsl%d" % i) for i in range(2)]
    R_aT, R_cvT, R_mg = Res("aT"), Res("cvT"), Res("mg")
    cw = sb("cw", [128, 16, 3], F32)
    R_cw = Res("cw")
    S.dma(cw[:], convw, "cw", writes=[R_cw])
    cct = [sb("cct%d" % i, [128, TB + 2], F32) for i in range(2)]
    cht = [sb("cht%d" % i, [128, TB + 2], F32) for i in range(2)]
    cbt = [sb("cbt%d" % i, [128, TB], F32) for i in range(2)]
    ut = [sb("ut%d" % i, [128, TB + 2], F32) for i in range(2)]
    yt = [sb("yt%d" % i, [128, TB], F32) for i in range(2)]
    RCC = [Res("cct%d" % i) for i in range(2)]
    RCH = [Res("cht%d" % i) for i in range(2)]
    RCB = [Res("cbt%d" % i) for i in range(2)]
    RU = [Res("ut%d" % i) for i in range(2)]
    RY = [Res("yt%d" % i) for i in range(2)]
    gat = [sb("gat%d" % i, [128, TB], F32) for i in range(2)]
    gbt = [sb("gbt%d" % i, [128, TB], F32) for i in range(2)]
    t1 = [sb("t1%d" % i, [128, TB], F32) for i in range(2)]
    t2 = [sb("t2%d" % i, [128, TB], F32) for i in range(2)]
    RGA = [Res("gat%d" % i) for i in range(2)]
    RGB = [Res("gbt%d" % i) for i in range(2)]
    RT1 = [Res("t1%d" % i) for i in range(2)]
    RT2 = [Res("t2%d" % i) for i in range(2)]
    xm = [sb("xm%d" % i, [128, TB], F32) for i in range(2)]
    stg2 = [sb("stgB%d" % i, [128, TB], F32) for i in range(2)]
    RXM = [Res("xm%d" % i) for i in range(2)]
    RST2 = [Res("stgB%d" % i) for i in range(2)]
    attnTv = attnT.rearrange("(k p) t -> p k t", p=128)
    featF = featT.bitcast(F32)
    wi = 0
    pi = 0
    for ti in range(NTOK // TB):
        t0 = ti * TB
        e0 = HALO + t0
        S.dma(aT[:, 0:8, :], attnTv[:, 0:8, t0:t0 + TB], "aT", writes=[R_aT])
        S.dma(aT[:, 8:16, :], attnTv[:, 8:16, t0:t0 + TB], "aT", writes=[R_aT])
        for k in range(16):
            q = k % 2
            S.dma(cct[q][:], featF[8192 + k * 128:8192 + (k + 1) * 128, e0 - 1:e0 + TB + 1], RCC[q].key, writes=[RCC[q]], q="act")
            S.dma(cht[q][:], featF[10240 + k * 128:10240 + (k + 1) * 128, e0 - 1:e0 + TB + 1], RCH[q].key, writes=[RCH[q]], q="act")
            S.dma(cbt[q][:], featF[6144 + k * 128:6144 + (k + 1) * 128, e0:e0 + TB], RCB[q].key, writes=[RCB[q]], q="act")
            S.tt(ut[q][:], cct[q][:], cht[q][:], ALU.mult, reads=[RCC[q], RCH[q]], writes=[RU[q]], eng="pool")
            if ti == 0:
                S.ts(ut[q][:, 0:1], ut[q][:, 0:1], flags[:, 0:1], None, ALU.mult, reads=[RU[q], R_const], writes=[RU[q]], eng="pool")
            if ti == NTOK // TB - 1:
                S.ts(ut[q][:, TB + 1:TB + 2], ut[q][:, TB + 1:TB + 2], flags[:, 1:2], None, ALU.mult, reads=[RU[q], R_const], writes=[RU[q]], eng="pool")
            S.ts(yt[q][:], ut[q][:, 0:TB], cw[:, k, 0:1], None, ALU.mult, reads=[RU[q], R_cw], writes=[RY[q]])
            S.stt(yt[q][:], ut[q][:, 1:TB + 1], cw[:, k, 1:2], yt[q][:], ALU.mult, ALU.add, reads=[RU[q], R_cw, RY[q]], writes=[RY[q]])
            S.stt(yt[q][:], ut[q][:, 2:TB + 2], cw[:, k, 2:3], yt[q][:], ALU.mult, ALU.add, reads=[RU[q], R_cw, RY[q]], writes=[RY[q]])
            S.tt(cvT[:, k, :], cbt[q][:], yt[q][:], ALU.mult, reads=[RCB[q], RY[q]], writes=[R_cvT])
        for cs in range(16):
            c0 = cs * 256
            sl = wi % 2
            wi += 1
            S.dma(wsl[sl][:, 0:16, :], w_ao[cs].rearrange("p (k n) -> p k n", n=256), RWS2[sl].key, writes=[RWS2[sl]])
            S.dma(wsl[sl][:, 16:32, :], w_co[cs].rearrange("p (k n) -> p k n", n=256), RWS2[sl].key, writes=[RWS2[sl]])
            for half in range(2):
                m = cs * 2 + half
                ba, bb_ = (1, 2) if pi % 2 == 0 else (3, 4)
                pi += 1
                for k in range(16):
                    S.mm(ps[ba][:, 0:TB], wsl[sl][:, k, half * 128:(half + 1) * 128], aT[:, k, :], k == 0, k == 15,
                         reads=[RWS2[sl], R_aT], writes=[RP[ba]])
                for k in range(16):
                    S.mm(ps[bb_][:, 0:TB], wsl[sl][:, 16 + k, half * 128:(half + 1) * 128], cvT[:, k, :], k == 0, k == 15,
                         reads=[RWS2[sl], R_cvT], writes=[RP[bb_]])
                q = m % 2
                S.dma(gat[q][:], featF[12288 + m * 128:12288 + (m + 1) * 128, e0:e0 + TB], RGA[q].key, writes=[RGA[q]], q="act")
                S.dma(gbt[q][:], featF[16384 + m * 128:16384 + (m + 1) * 128, e0:e0 + TB], RGB[q].key, writes=[RGB[q]], q="act")
                S.act(gat[q][:], gat[q][:], AF.Sigmoid, reads=[RGA[q]], writes=[RGA[q]])
                S.act(gbt[q][:], gbt[q][:], AF.Sigmoid, reads=[RGB[q]], writes=[RGB[q]])
                S.tt(t1[q][:], ps[ba][:, 0:TB], gat[q][:], ALU.mult, reads=[RP[ba], RGA[q]], writes=[RT1[q]])
                S.tt(t2[q][:], ps[bb_][:, 0:TB], gbt[q][:], ALU.mult, reads=[RP[bb_], RGB[q]], writes=[RT2[q]])
                S.tt(mg[:, m, :], t1[q][:], t2[q][:], ALU.add, reads=[RT1[q], RT2[q]], writes=[R_mg])
        for cs in range(16):
            c0 = cs * 256
            sl = wi % 2
            wi += 1
            for kq in range(2):
                S.dma(wsl[sl][:, kq * 16:(kq + 1) * 16, :],
                      w_o[cs][:, kq * 4096:(kq + 1) * 4096].rearrange("p (k n) -> p k n", n=256), RWS2[sl].key, writes=[RWS2[sl]])
            for half in range(2):
                m = cs * 2 + half
                bo = 5 + m % 2
                for k in range(32):
                    S.mm(ps[bo][:, 0:TB], wsl[sl][:, k, half * 128:(half + 1) * 128], mg[:, k, :], k == 0, k == 31,
                         reads=[RWS2[sl], R_mg], writes=[RP[bo]])
                q = m % 2
                S.dma(xm[q][:], xT[m * 128:(m + 1) * 128, e0:e0 + TB], RXM[q].key, writes=[RXM[q]], q="act")
                S.stt(stg2[q][:], ps[bo][:, 0:TB], gate1[:, m:m + 1], xm[q][:], ALU.mult, ALU.add,
                      reads=[RP[bo], RXM[q], R_mod], writes=[RST2[q]])
                S.dma(x1T[m * 128:(m + 1) * 128, t0:t0 + TB], stg2[q][:], RST2[q].key, reads=[RST2[q]], q="act")
    S.barrier()
    esC.close()
    if upto < 4:
        S.emit()
        return nc

    esD = ExitStack()
    sb = lambda n, sh, dt: esD.enter_context(nc.sbuf_tensor(n, sh, dt))
    TC = 256
    NSB = 16
    accmem = sb("accmem", [128, 32 * TC], F32)
    acc = accmem[:].rearrange("p (k t) -> p k t", k=32)
    P_tok = [accmem[:, sub * 4096:(sub + 1) * 4096] for sub in range(2)]
    acc2 = accmem[:].rearrange("p (s d) -> p s d", s=2)
    xmc = [sb("xmc%d" % i, [128, TC], F32) for i in range(2)]
    RXMC = [Res("xmc%d" % i) for i in range(2)]
    RACC = [Res("acc%d" % m) for m in range(32)]
    h2T = sb("h2T", [128, 32, TC], F32R)
    R_h2 = Res("h2T")
    NUS, NVS = 3, 5
    us = [sb("us%d" % i, [128, 32, 128], F32R) for i in range(NUS)]
    RUS = [Res("us%d" % i) for i in range(NUS)]
    vs_ = [sb("vs%d" % i, [128, 2, 512], F32R) for i in range(NVS)]
    RVS = [Res("vs%d" % i) for i in range(NVS)]
    NTILE_C = NTOK // 256
    ust = {"next": 0, "use": 0}
    vst = {"next": 0, "use": 0}

    def u_prefetch(upto_i):
        while ust["next"] <= upto_i and ust["next"] < NTILE_C * 144:
            i = ust["next"]
            ust["next"] += 1
            r = i % 144
            src = w_q[r] if r < 16 else UT[r - 16]
            sl_ = i % NUS
            S.dma(us[sl_][:], src.rearrange("p (k n) -> p k n", n=128), RUS[sl_].key, writes=[RUS[sl_]])

    def u_use():
        i = ust["use"]
        ust["use"] += 1
        u_prefetch(i + NUS - 1)
        return i % NUS

    def v_prefetch(upto_j):
        while vst["next"] <= upto_j and vst["next"] < NTILE_C * 16 * 32:
            j = vst["next"]
            vst["next"] += 1
            sbk_, r_ = (j // 32) % 16, j % 32
            sl_ = j % NVS
            S.dma(vs_[sl_][:], EV[sbk_, r_ // 4, r_ % 4].rearrange("p (c n) -> p c n", n=512), RVS[sl_].key, writes=[RVS[sl_]])

    def v_use():
        j = vst["use"]
        vst["use"] += 1
        v_prefetch(j + NVS - 1)
        return j % NVS
    zbuf = sb("zbuf", [128, 4096], F32)
    qTs = zbuf[:].rearrange("p (g t) -> p g t", g=16)
    R_qT = Res("qTs")
    GAT = sb("GAT", [128, 8, TC], F32R)
    R_GAT = Res("GAT")
    s12 = [sb("s12%d" % i, [128, 16, 128], F32) for i in range(2)]
    R_s12 = [Res("s12%d" % i) for i in range(2)]
    Gacc = [sb("Gacc%d" % i, [128, 8, 128], F32) for i in range(2)]
    R_G = [Res("Gacc%d" % i) for i in range(2)]
    zt = [zbuf[:, i * 1024:(i + 1) * 1024].rearrange("p (c n) -> p c n", n=128) for i in range(2)]
    ezt = [zbuf[:, (2 + i) * 1024:(3 + i) * 1024].rearrange("p (c n) -> p c n", n=128) for i in range(2)]
    R_z = [Res("zt%d" % i) for i in range(2)]
    R_ez = [Res("ezt%d" % i) for i in range(2)]
    AgT = [sb("AgT%d" % i, [128, TC], F32) for i in range(2)]
    R_Ag = [Res("AgT%d" % i) for i in range(2)]
    keys_sb = sb("keys_sb", [128, 16, 128], F32)
    R_keys = Res("keys")
    S.dma(keys_sb[:], keysT, "keys", writes=[R_keys])
    sq2 = [sb("sqc%d" % i, [128, TC], F32R) for i in range(2)]
    RSQ2 = [Res("sqc%d" % i) for i in range(2)]
    rstd2 = sb("rstd2", [128, TC], F32)
    R_rstd2 = Res("rstd2")
    ntmp = [sb("ntmp%d" % i, [128, TC], F32) for i in range(2)]
    RNT = [Res("ntmp%d" % i) for i in range(2)]
    vtop = [sb("vtop%d" % i, [128, 16, 16], F32) for i in range(2)]
    mrt = sb("mrt", [128, 256], F32)
    cand = zbuf[:, 0:2048].rearrange("p (h a b) -> p h a b", h=8, a=16)
    tsv = [sb("tsv%d" % i, [128, 8, 16], F32) for i in range(2)]
    sm2 = [sb("sm2%d" % i, [128, 4, 8], F32) for i in range(2)]
    extmp = sb("extmp", [128, 8, 16], F32)
    R_top = [Res("top%d" % i) for i in range(2)]
    R_mrt, R_cand, R_ext = Res("mrt"), Res("cand"), Res("extmp")
    stg3 = [sb("stgC%d" % i, [128, TC], F32) for i in range(2)]
    RST3 = [Res("stgC%d" % i) for i in range(2)]
    x1Tv = x1T.rearrange("(k p) t -> p k t", p=128)
    ui = 0
    vi = 0
    zi = 0
    ai = 0
    for ti in range(NTOK // TC):
        t0 = ti * TC
        for kq in range(4):
            S.dma(acc[:, kq * 8:(kq + 1) * 8, :], x1Tv[:, kq * 8:(kq + 1) * 8, t0:t0 + TC], "accl%d" % kq,
                  writes=RACC[kq * 8:(kq + 1) * 8])
        for k in range(32):
            q = k % 2
            S.act(sq2[q][:], acc[:, k, :], AF.Square, reads=[RACC[k]], writes=[RSQ2[q]])
            S.mm(ps[0][:, 0:TC], ones[:], sq2[q][:], k == 0, k == 31, reads=[RSQ2[q], R_const], writes=[RP[0]])
        S.ts(rstd2[:], ps[0][:, 0:TC], 1.0 / D, EPS, ALU.mult, ALU.add, reads=[RP[0]], writes=[R_rstd2])
        S.act(rstd2[:], rstd2[:], AF.Sqrt, reads=[R_rstd2], writes=[R_rstd2])
        S.recip(rstd2[:], rstd2[:], reads=[R_rstd2], writes=[R_rstd2])
        for k in range(32):
            q = k % 2
            S.tt(ntmp[q][:], acc[:, k, :], rstd2[:], ALU.mult, reads=[RACC[k], R_rstd2], writes=[RNT[q]], eng="pool")
            S.ts(h2T[:, k, :], ntmp[q][:], gs2[:, k:k + 1], sh2[:, k:k + 1], ALU.mult, ALU.add,
                 reads=[RNT[q], R_mod], writes=[R_h2])
        for g in range(16):
            sl = u_use()
            bq = 1 + g % 2
            for k in range(32):
                S.mm(ps[bq][:, 0:TC], us[sl][:, k, :], h2T[:, k, :], k == 0, k == 31, reads=[RUS[sl], R_h2], writes=[RP[bq]])
            S.cp(qTs[:, g, :], ps[bq][:, 0:TC], reads=[RP[bq]], writes=[R_qT, R_z[0], R_z[1], R_ez[0], R_ez[1]], eng=("act" if g % 2 else "dve"))
        for u in range(2):
            for gq in range(4):
                bank = 3 + gq % 2
                for gg in range(4):
                    g = gq * 4 + gg
                    S.mm(ps[bank][:, gg * 128:(gg + 1) * 128], qTs[:, g, u * 128:(u + 1) * 128], keys_sb[:, g, :], True, True,
                         reads=[R_qT, R_z[0], R_z[1], R_ez[0], R_ez[1], R_keys], writes=[RP[bank]])
                S.cp(s12[u][:, gq * 4:(gq + 1) * 4, :], ps[bank][:, 0:512].rearrange("p (a b) -> p a b", a=4),
                     reads=[RP[bank]], writes=[R_s12[u]], eng="act")
        for u in range(2):
            for g in range(16):
                S.max8(vtop[u][:, g, 0:8], s12[u][:, g, :], reads=[R_s12[u]], writes=[R_top[u]])
                S.mrep(mrt[:, 0:128], vtop[u][:, g, 0:8], s12[u][:, g, :], reads=[R_s12[u], R_top[u]], writes=[R_mrt])
                S.max8(vtop[u][:, g, 8:16], mrt[:, 0:128], reads=[R_mrt], writes=[R_top[u]])
            v1 = vtop[u][:, 0:16:2, :].unsqueeze(3).to_broadcast([128, 8, 16, 16])
            v2 = vtop[u][:, 1:16:2, :].unsqueeze(2).to_broadcast([128, 8, 16, 16])
            S.tt(cand, v1, v2, ALU.add, reads=[R_top[u]], writes=[R_cand, R_z[0], R_z[1], R_qT])
            for hh in range(8):
                cflat = cand[:, hh, :, :].rearrange("p a b -> p (a b)")
                S.max8(tsv[u][:, hh, 0:8], cflat, reads=[R_cand, R_z[0], R_z[1]], writes=[R_top[u]])
                S.mrep(mrt[:], tsv[u][:, hh, 0:8], cflat, reads=[R_cand, R_z[0], R_z[1], R_top[u]], writes=[R_mrt])
                S.max8(tsv[u][:, hh, 8:16], mrt[:], reads=[R_mrt], writes=[R_top[u]])
            S.ts(sm2[u][:, 0, :], tsv[u][:, :, 0], -1.0, None, ALU.mult, reads=[R_top[u]], writes=[R_top[u]])
            S.cp(sm2[u][:, 1, :], tsv[u][:, :, 15], reads=[R_top[u]], writes=[R_top[u]])
            S.tt(extmp[:], tsv[u][:], sm2[u][:, 0, :].unsqueeze(2).to_broadcast([128, 8, 16]), ALU.add,
                 reads=[R_top[u]], writes=[R_ext])
            S.act(extmp[:], extmp[:], AF.Exp, reads=[R_ext], writes=[R_ext])
            S.rsum(sm2[u][:, 2, :], extmp[:], reads=[R_ext], writes=[R_top[u]])
            S.recip(sm2[u][:, 3, :], sm2[u][:, 2, :], reads=[R_top[u]], writes=[R_top[u]])
        def G_iter(sbk_, u, hh):
            nonlocal zi
            cb_ = sbk_ * 8
            zb = zi % 2
            zi += 1
            s2b = s12[u][:, 2 * hh + 1, :].unsqueeze(1).to_broadcast([128, 8, 128])
            s1b = s12[u][:, 2 * hh, cb_:cb_ + 8].unsqueeze(2).to_broadcast([128, 8, 128])
            S.tt(zt[zb][:], s2b, s1b, ALU.add, reads=[R_s12[u]], writes=[R_z[zb]], eng="pool")
            S.act(ezt[zb][:], zt[zb][:], AF.Exp, bias=sm2[u][:, 0, hh:hh + 1], reads=[R_z[zb], R_top[u]], writes=[R_ez[zb]])
            S.stt(ezt[zb][:], zt[zb][:], sm2[u][:, 1, hh:hh + 1], ezt[zb][:], ALU.is_ge, ALU.mult,
                  reads=[R_z[zb], R_ez[zb], R_top[u]], writes=[R_ez[zb]])
            if hh == 0:
                S.ts(Gacc[u][:], ezt[zb][:], sm2[u][:, 3, hh:hh + 1], None, ALU.mult,
                     reads=[R_ez[zb], R_top[u]], writes=[R_G[u]])
            else:
                S.stt(Gacc[u][:], ezt[zb][:], sm2[u][:, 3, hh:hh + 1], Gacc[u][:], ALU.mult, ALU.add,
                      reads=[R_ez[zb], R_top[u], R_G[u]], writes=[R_G[u]])

        for u in range(2):
            for hh in range(8):
                G_iter(0, u, hh)
        for sbk in range(NSB):
            v_prefetch(((ti * NSB) + sbk) * 32 + NVS - 1)
            for cc in range(8):
                sl = u_use()
                ba = 1 + ai % 2
                aq = ai % 2
                ai += 1
                for k in range(32):
                    S.mm(ps[ba][:, 0:TC], us[sl][:, k, :], h2T[:, k, :], k == 0, k == 31, reads=[RUS[sl], R_h2], writes=[RP[ba]])
                S.act(AgT[aq][:], ps[ba][:, 0:TC], AF.Gelu_apprx_tanh, reads=[RP[ba]], writes=[R_Ag[aq]])
                for u in range(2):
                    S.tr(ps[5][:, u * 128:(u + 1) * 128], Gacc[u][:, cc, :], ident[:], reads=[R_G[u], R_const], writes=[RP[5]])
                S.tt(GAT[:, cc, :], AgT[aq][:], ps[5][:, 0:TC], ALU.mult, reads=[R_Ag[aq], RP[5]], writes=[R_GAT])
            giters = [(u, hh) for u in range(2) for hh in range(8)] if sbk + 1 < NSB else []
            for dg in range(8):
                banks = (6, 7) if dg % 2 == 0 else (3, 4)
                for ccp in range(4):
                    sl = v_use()
                    for c2 in range(2):
                        cc = ccp * 2 + c2
                        for sub in range(2):
                            S.mm(ps[banks[sub]][:], GAT[:, cc, sub * 128:(sub + 1) * 128], vs_[sl][:, c2, :], cc == 0, cc == 7,
                                 reads=[R_GAT, RVS[sl]], writes=[RP[banks[sub]]])
                for sub in range(2):
                    rr = [RACC[sub * 16 + 2 * dg], RACC[sub * 16 + 2 * dg + 1]]
                    dst = P_tok[sub][:, dg * 512:(dg + 1) * 512]
                    if sbk == 0:
                        S.cp(dst, ps[banks[sub]][:], reads=[RP[banks[sub]]], writes=rr)
                    else:
                        S.tt(dst, ps[banks[sub]][:], dst, ALU.add, reads=[RP[banks[sub]]] + rr, writes=rr)
                for _ in range(2):
                    if giters:
                        G_iter(sbk + 1, *giters.pop(0))
        for m in range(32):
            q = m % 2
            S.dma(xmc[q][:], x1T[m * 128:(m + 1) * 128, t0:t0 + TC], RXMC[q].key, writes=[RXMC[q]], q="act")
            rr = [RACC[m // 2], RACC[16 + m // 2]]
            for sub in range(2):
                S.tr(ps[5][:, sub * 128:(sub + 1) * 128], P_tok[sub][:, m * 128:(m + 1) * 128], ident[:], reads=[rr[sub], R_const], writes=[RP[5]])
            x2v = acc2[:, :, m * 128:(m + 1) * 128]
            S.stt(x2v, ps[5][:, 0:TC].rearrange("p (s t) -> p s t", s=2), gate2[:, m:m + 1],
                  xmc[q][:].rearrange("p (s t) -> p s t", s=2), ALU.mult, ALU.add,
                  reads=[RP[5], RXMC[q], R_mod] + rr, writes=rr)
            S.act(sq2[q][:].rearrange("p (s t) -> p s t", s=2), x2v, AF.Square, reads=rr, writes=[RSQ2[q]])
            S.mm(ps[0][:, 0:TC], ones[:], sq2[q][:], m == 0, m == 31, reads=[RSQ2[q], R_const], writes=[RP[0]])
        S.ts(rstd2[:], ps[0][:, 0:TC], 1.0 / D, EPS, ALU.mult, ALU.add, reads=[RP[0]], writes=[R_rstd2])
        S.act(rstd2[:], rstd2[:], AF.Sqrt, reads=[R_rstd2], writes=[R_rstd2])
        S.recip(rstd2[:], rstd2[:], reads=[R_rstd2], writes=[R_rstd2])
        for m in range(32):
            q = m % 2
            rr = [RACC[m // 2], RACC[16 + m // 2]]
            x2v = acc2[:, :, m * 128:(m + 1) * 128]
            S.tt(ntmp[q][:].rearrange("p (s t) -> p s t", s=2), x2v, rstd2[:].rearrange("p (s t) -> p s t", s=2), ALU.mult,
                 reads=rr + [R_rstd2], writes=[RNT[q]], eng="pool")
            S.ts(stg3[q][:], ntmp[q][:], gfs[:, m:m + 1], None, ALU.mult, reads=[RNT[q], R_const], writes=[RST3[q]])
            S.dma(outT[m * 128:(m + 1) * 128, t0:t0 + TC], stg3[q][:], RST3[q].key, reads=[RST3[q]], q="act")
    S.barrier()
    esD.close()
    S.emit()
    return nc


def _bias_tables(rpb_l, top_edge, bot_edge):
    H = 16
    qc = np.arange(64)
    cstart = np.clip(qc - 8, 0, 48)

    def table(width_rows, row_off, qrows_abs, rs_abs):
        W = width_rows * 64
        tab = np.full((H, 128, W), NEG, np.float32)
        for qr in range(2):
            rs = rs_abs[qr]
            for i in range(8):
                krow = rs + i
                wr = krow - row_off
                if wr < 0 or wr >= width_rows:
                    continue
                dr = krow - qr + 7
                for x in range(64):
                    ys = cstart[x] + np.arange(16)
                    dc = ys - x + 15
                    tab[:, qr * 64 + x, wr * 64 + ys] = rpb_l[:, dr, dc]
        return tab

    gen = table(10, -4, None, (-4, -3))
    if top_edge:
        top = table(12, -4, None, (0, 0))
    else:
        top = table(12, -4, None, (-4, -3))
    if bot_edge:
        bot = table(12, -6, None, (-6, -6))
    else:
        bot = table(12, -6, None, (-4, -3))
    top1 = table(10, -4, None, (-2, -2)) if top_edge else gen
    bot14 = table(10, -4, None, (-4, -4)) if bot_edge else gen
    return gen, top, bot, top1, bot14


_NC_CACHE = {}


def _prep_inputs(inp):
    x = np.asarray(inp["x"], np.float32)
    c = np.asarray(inp["c"], np.float32)
    l = 0
    shared = {
        "w_ada": np.ascontiguousarray(inp["w_ada"][l], np.float32),
        "b_ada": np.ascontiguousarray(inp["b_ada"][l].reshape(1, -1), np.float32),
        "g1": np.ascontiguousarray(inp["norm1_g"][l].reshape(32, 128).T, np.float32),
        "g2": np.ascontiguousarray(inp["norm2_g"][l].reshape(32, 128).T, np.float32),
        "gf": np.ascontiguousarray(np.asarray(inp["norm_f_g"]).reshape(32, 128).T, np.float32),
        "w_in": np.ascontiguousarray(np.asarray(inp["w_in"][l], np.float32).reshape(32, 128, 80, 256).transpose(2, 1, 0, 3)).reshape(80, 128, 8192),
        "convw": np.ascontiguousarray(np.asarray(inp["conv_w"][l]).reshape(3, 16, 128).transpose(2, 1, 0), np.float32),
        "w_ao": np.ascontiguousarray(np.asarray(inp["w_attn_out"][l], np.float32).reshape(16, 128, 16, 256).transpose(2, 1, 0, 3)).reshape(16, 128, 4096),
        "w_co": np.ascontiguousarray(np.asarray(inp["w_conv_out"][l], np.float32).reshape(16, 128, 16, 256).transpose(2, 1, 0, 3)).reshape(16, 128, 4096),
        "w_o": np.ascontiguousarray(np.asarray(inp["w_o"][l], np.float32).reshape(32, 128, 16, 256).transpose(2, 1, 0, 3)).reshape(16, 128, 8192),
        "w_q": np.ascontiguousarray(np.asarray(inp["w_q_peer"][l], np.float32).reshape(32, 128, 16, 128).transpose(2, 1, 0, 3)).reshape(16, 128, 4096),
        "UT": np.ascontiguousarray(np.asarray(inp["expert_u"][l], np.float32).reshape(128, 128, 32, 128).transpose(0, 3, 2, 1)).reshape(128, 128, 4096),
        "EV": np.ascontiguousarray(np.asarray(inp["expert_v"][l], np.float32).reshape(16, 4, 2, 128, 8, 512).transpose(0, 4, 1, 3, 2, 5)).reshape(16, 8, 4, 128, 1024),
        "ident": np.eye(128, dtype=np.float32),
    }
    k1 = np.asarray(inp["sub_keys_1"][l])
    k2 = np.asarray(inp["sub_keys_2"][l])
    kk = np.stack([k1, k2], axis=1).reshape(16, 128, 128)
    shared["keysT"] = np.ascontiguousarray(kk.transpose(2, 0, 1), np.float32)
    rpb_l = np.asarray(inp["rpb"][l], np.float32)
    tabs = {}
    maps = []
    for core in range(8):
        b, qd = core // 4, core % 4
        t0 = qd * NTOK
        xe = np.zeros((NEXT, D), np.float32)
        lo, hi = t0 - HALO, t0 + NTOK + HALO
        slo, shi = max(lo, 0), min(hi, 8192)
        xe[slo - lo: shi - lo] = x[b, slo:shi]
        m = dict(shared)
        m["xT"] = np.ascontiguousarray(xe.T)
        m["cT"] = np.ascontiguousarray(c[b].reshape(32, 128).T, np.float32)
        te, be = (qd == 0), (qd == 3)
        if (te, be) not in tabs:
            tabs[(te, be)] = _bias_tables(rpb_l, te, be)
        g_, t_, b_, t1_, b14_ = tabs[(te, be)]
        m["biasg"], m["biast"], m["biasb"], m["bias1"], m["bias14"] = g_, t_, b_, t1_, b14_
        fl = np.ones((128, 2), np.float32)
        if te:
            fl[:, 0] = 0.0
        if be:
            fl[:, 1] = 0.0
        m["flags"] = fl
        maps.append(m)
    return maps


def kernel(**inputs):
    maps = _prep_inputs(inputs)
    if "nc" not in _NC_CACHE:
        _NC_CACHE["nc"] = build_nc()
    nc = _NC_CACHE["nc"]
    res = run_bass_kernel_spmd(nc, maps, core_ids=list(range(8)))
    out = np.zeros((2, 8192, D), np.float32)
    for core in range(8):
        b, qd = core // 4, core % 4
        out[b, qd * NTOK:(qd + 1) * NTOK] = res.results[core]["outT"].T
    return out
```

```python
import numpy as np
import concourse.bass as bass
import concourse.mybir as mybir
from concourse.bass_utils import run_bass_kernel_spmd
from contextlib import ExitStack

F32 = mybir.dt.float32
F32R = mybir.dt.float32r
AF = mybir.ActivationFunctionType
ALU = mybir.AluOpType
AX = mybir.AxisListType

D = 4096
NTOK = 2048
HALO = 256
NEXT = NTOK + 2 * HALO
INC = 20480
NEG = -1.0e30
EPS = 1e-6
SCALE = 128 ** -0.5


class Res:
    __slots__ = ("name", "w", "r", "key")

    def __init__(self, name, key=None):
        self.name = name
        self.w = {}
        self.r = {}
        self.key = key if key is not None else name


class Op:
    __slots__ = ("eng", "fn", "deps", "need", "val", "dma_key", "idx")


class Sched:
    ENG = ("pe", "act", "dve", "pool", "sp")

    def __init__(self, nc):
        self.nc = nc
        self.ops = {e: [] for e in self.ENG}
        self.dma_cnt = {}
        self.n = 0
        self.strict = True

    def op(self, eng, fn, reads=(), writes=(), dma_key=None):
        o = Op()
        o.eng = eng
        o.fn = fn
        o.need = False
        o.val = None
        o.dma_key = dma_key
        o.idx = self.n
        self.n += 1
        deps = {}
        for r in reads:
            for d in r.w.values():
                deps[id(d)] = d
        for w in writes:
            for d in w.w.values():
                deps[id(d)] = d
            for d in w.r.values():
                deps[id(d)] = d
        dl = []
        for d in deps.values():
            if d.dma_key is None and d.eng == eng and dma_key is None and (eng == "pe" or not self.strict):
                continue
            if dma_key is not None and d.dma_key == dma_key:
                continue
            d.need = True
            dl.append(d)
        o.deps = dl
        if dma_key is not None:
            c = self.dma_cnt.get(dma_key, 0) + 1
            self.dma_cnt[dma_key] = c
            o.val = 16 * c
            key = "dma:" + dma_key
        else:
            key = eng
        for r in reads:
            r.r[key] = o
        for w in writes:
            w.w = {key: o}
            w.r = {}
        self.ops[eng].append(o)
        return o

    def barrier(self):
        lasts = []
        for e in self.ENG:
            for o_ in reversed(self.ops[e]):
                if o_.fn is not None and o_.dma_key is None:
                    lasts.append(o_)
                    break
        dmas = dict(self.dma_cnt)
        for e in self.ENG:
            o = Op()
            o.eng = e
            o.fn = None
            o.need = False
            o.val = None
            o.dma_key = None
            o.idx = self.n
            self.n += 1
            dl = []
            for l in lasts:
                if l.eng != e and l.dma_key is None:
                    l.need = True
                    dl.append(l)
            o.deps = dl + [("dma", k, 16 * c) for k, c in dmas.items()]
            self.ops[e].append(o)

    def emit(self):
        nc = self.nc
        for e in self.ENG:
            c = 0
            for o in self.ops[e]:
                if o.dma_key is None and o.need:
                    c += 1
                    o.val = c
        sems = {}

        def sem(k):
            if k not in sems:
                sems[k] = nc.alloc_semaphore(name="s_" + k.replace(":", "_"))
            return sems[k]

        handles = {"pe": nc.tensor, "act": nc.scalar, "dve": nc.vector, "pool": nc.gpsimd, "sp": nc.sync}

        def run(e, h):
            waited = {}
            for o in self.ops[e]:
                for d in o.deps:
                    if isinstance(d, tuple):
                        k, v = "dma:" + d[1], d[2]
                    elif d.dma_key is not None:
                        k, v = "dma:" + d.dma_key, d.val
                    else:
                        k, v = d.eng, d.val
                    if waited.get(k, 0) < v:
                        h.wait_ge(sem(k), v)
                        waited[k] = v
                if o.fn is None:
                    continue
                ins = o.fn(h)
                if o.dma_key is not None:
                    ins.then_inc(sem("dma:" + o.dma_key), 16)
                elif o.need:
                    ins.then_inc(sem(e), 1)

        with nc.Block() as block:
            @block.tensor
            def _(h):
                run("pe", h)

            @block.scalar
            def _(h):
                run("act", h)

            @block.vector
            def _(h):
                run("dve", h)

            @block.gpsimd
            def _(h):
                run("pool", h)

            @block.sync
            def _(h):
                run("sp", h)

    def dma(self, out, in_, key, reads=(), writes=(), q="sp"):
        return self.op(q, lambda h: h.dma_start(out=out, in_=in_), reads, writes, dma_key=key)

    def mm(self, out, lhsT, rhs, start, stop, reads=(), writes=()):
        return self.op("pe", lambda h: h.matmul(out, lhsT, rhs, start=start, stop=stop, skip_group_check=True), reads, writes)

    def tr(self, out, in_, ident, reads=(), writes=()):
        return self.op("pe", lambda h: h.transpose(out, in_, ident), reads, writes)

    def act(self, out, in_, func, reads=(), writes=(), eng="act", **kw):
        return self.op(eng, lambda h: h.activation(out=out, in_=in_, func=func, **kw), reads, writes)

    def tt(self, out, in0, in1, op, reads=(), writes=(), eng="dve"):
        return self.op(eng, lambda h: h.tensor_tensor(out=out, in0=in0, in1=in1, op=op), reads, writes)

    def ts(self, out, in0, s1, s2, op0, op1=None, reads=(), writes=(), eng="dve"):
        if op1 is None:
            return self.op(eng, lambda h: h.tensor_scalar(out=out, in0=in0, scalar1=s1, scalar2=None, op0=op0), reads, writes)
        return self.op(eng, lambda h: h.tensor_scalar(out=out, in0=in0, scalar1=s1, scalar2=s2, op0=op0, op1=op1), reads, writes)

    def stt(self, out, in0, scalar, in1, op0, op1, reads=(), writes=()):
        return self.op("dve", lambda h: h.scalar_tensor_tensor(out=out, in0=in0, scalar=scalar, in1=in1, op0=op0, op1=op1), reads, writes)

    def cp(self, out, in_, reads=(), writes=(), eng="dve"):
        if eng == "act":
            return self.op("act", lambda h: h.copy(out=out, in_=in_), reads, writes)
        return self.op(eng, lambda h: h.tensor_copy(out=out, in_=in_), reads, writes)

    def rmax(self, out, in_, reads=(), writes=()):
        return self.op("dve", lambda h: h.reduce_max(out=out, in_=in_, axis=AX.X), reads, writes)

    def rsum(self, out, in_, reads=(), writes=()):
        return self.op("dve", lambda h: h.reduce_sum(out=out, in_=in_, axis=AX.X), reads, writes)

    def recip(self, out, in_, reads=(), writes=()):
        return self.op("dve", lambda h: h.reciprocal(out=out, in_=in_), reads, writes)

    def max8(self, out, in_, reads=(), writes=()):
        return self.op("dve", lambda h: h.max(out=out, in_=in_), reads, writes)

    def mrep(self, out, rep, vals, reads=(), writes=()):
        return self.op("dve", lambda h: h.match_replace(out=out, in_to_replace=rep, in_values=vals, imm_value=NEG), reads, writes)

    def gen(self, eng, fn, reads=(), writes=()):
        return self.op(eng, fn, reads, writes)


def build_nc(upto=99, debug=False):
    nc = bass.Bass("TRN2", target_bir_lowering=False)
    nc.dge_precook = False
    S = Sched(nc)
    skind = "ExternalOutput" if debug else "Internal"

    def din(name, shape, dt=F32):
        return nc.dram_tensor(name, list(shape), dt, kind="ExternalInput").ap()

    xT = din("xT", [D, NEXT])
    cT = din("cT", [128, 32], F32R)
    w_ada = din("w_ada", [D, 6 * D], F32R)
    b_ada = din("b_ada", [1, 6 * D])
    g1 = din("g1", [128, 32])
    g2 = din("g2", [128, 32])
    gf = din("gf", [128, 32])
    w_in = din("w_in", [80, 128, 32 * 256], F32R)
    biasg = din("biasg", [16, 128, 640])
    bias1 = din("bias1", [16, 128, 640])
    bias14 = din("bias14", [16, 128, 640])
    biast = din("biast", [16, 128, 768])
    biasb = din("biasb", [16, 128, 768])
    convw = din("convw", [128, 16, 3])
    w_ao = din("w_ao", [16, 128, 16 * 256], F32R)
    w_co = din("w_co", [16, 128, 16 * 256], F32R)
    w_o = din("w_o", [16, 128, 32 * 256], F32R)
    w_q = din("w_q", [16, 128, 32 * 128], F32R)
    keysT = din("keysT", [128, 16, 128])
    UT = din("UT", [128, 128, 32 * 128], F32R)
    EV = din("EV", [16, 8, 4, 128, 2 * 512], F32R)
    ident_d = din("ident", [128, 128])
    flags_d = din("flags", [128, 2])
    outT = nc.dram_tensor("outT", [D, NTOK], F32, kind="ExternalOutput").ap()
    featT = nc.dram_tensor("featT", [INC, NEXT], F32R, kind=skind).ap()
    vtok = nc.dram_tensor("vtok", [NEXT, 2048], F32R, kind=skind).ap()
    attnT = nc.dram_tensor("attnT", [2048, NTOK], F32R, kind=skind).ap()
    x1T = nc.dram_tensor("x1T", [D, NTOK], F32, kind=skind).ap()
    modd = nc.dram_tensor("modd", [128, 6 * 32], F32, kind=skind).ap()

    sb = nc.alloc_sbuf_tensor
    mod = sb("mod", [128, 6, 32], F32)
    gs1 = sb("gs1", [128, 32], F32)
    gs2 = sb("gs2", [128, 32], F32)
    g1s = sb("g1s", [128, 32], F32)
    g2s = sb("g2s", [128, 32], F32)
    gfs = sb("gfs", [128, 32], F32)
    ones = sb("ones", [128, 128], F32R)
    ident = sb("identsb", [128, 128], F32)
    flags = sb("flagssb", [128, 2], F32)
    one1 = sb("one1", [1, 1], F32)
    R_const = Res("const")
    ps = [nc.alloc_psum_tensor("ps%d" % i, [128, 512], F32) for i in range(8)]
    RP = [Res("ps%d" % i) for i in range(8)]

    S.dma(g1s[:], g1, "const", writes=[R_const])
    S.dma(g2s[:], g2, "const", writes=[R_const])
    S.dma(gfs[:], gf, "const", writes=[R_const])
    S.dma(ident[:], ident_d, "const", writes=[R_const])
    S.dma(flags[:], flags_d, "const", writes=[R_const])
    onesf = sb("onesf", [128, 128], F32)
    S.gen("dve", lambda h: h.memset(onesf[:], 1.0), writes=[R_const])
    S.cp(ones[:], onesf[:], reads=[R_const], writes=[R_const])
    S.gen("dve", lambda h: h.memset(one1[:], 1.0), writes=[R_const])

    es0 = ExitStack()
    sb0 = lambda n, sh, dt: es0.enter_context(nc.sbuf_tensor(n, sh, dt))
    cTs = sb0("cTs", [128, 32], F32R)
    was = [sb0("wa%d" % i, [128, 4096], F32R) for i in range(3)]
    RWA = [Res("wa%d" % i) for i in range(3)]
    rowb = sb0("rowb", [1, 4096], F32)
    row = sb0("row", [1, 4096], F32)
    R_cT, R_rowb, R_row, R_mod = Res("cT"), Res("rowb"), Res("row"), Res("mod")
    S.dma(cTs[:], cT, "cT", writes=[R_cT])
    li = 0
    for g in range(6):
        S.dma(rowb[:], b_ada[:, g * 4096:(g + 1) * 4096], "rowb", writes=[R_rowb])
        for k in range(32):
            sl = li % 3
            li += 1
            for hh in range(2):
                S.dma(was[sl][:, hh * 2048:(hh + 1) * 2048],
                      w_ada[k * 128:(k + 1) * 128, g * 4096 + hh * 2048: g * 4096 + (hh + 1) * 2048],
                      "wa%d" % sl, writes=[RWA[sl]])
            for b in range(8):
                S.mm(ps[b][0:1, :], cTs[:, k:k + 1], was[sl][:, b * 512:(b + 1) * 512], k == 0, k == 31,
                     reads=[R_cT, RWA[sl]], writes=[RP[b]])
        for b in range(8):
            S.tt(row[:, b * 512:(b + 1) * 512], ps[b][0:1, :], rowb[:, b * 512:(b + 1) * 512], ALU.add,
                 reads=[RP[b], R_rowb], writes=[R_row])
        for j in range(32):
            S.mm(ps[0][:, j:j + 1], row[0:1, j * 128:(j + 1) * 128], one1[0:1, 0:1], True, True,
                 reads=[R_row, R_const], writes=[RP[0]])
        S.cp(mod[:, g, :], ps[0][:, 0:32], reads=[RP[0]], writes=[R_mod])
    S.stt(gs1[:], mod[:, 1, :], 1.0, g1s[:], ALU.add, ALU.mult, reads=[R_mod, R_const], writes=[R_mod])
    S.stt(gs2[:], mod[:, 4, :], 1.0, g2s[:], ALU.add, ALU.mult, reads=[R_mod, R_const], writes=[R_mod])
    if debug:
        S.dma(modd, mod[:].rearrange("p a b -> p (a b)"), "modd", reads=[R_mod])
    sh1, gate1, sh2, gate2 = mod[:, 0, :], mod[:, 2, :], mod[:, 3, :], mod[:, 5, :]
    S.barrier()
    es0.close()
    if upto < 1:
        S.emit()
        return nc

    def norm_tile(src_load, T, dst, gs, sh, nk=32):
        for kq in range(8):
            sl = kq % 2
            src_load(kq, xs[sl], RXS[sl])
            for kk in range(4):
                k = kq * 4 + kk
                q = k % 2
                S.act(sq[q][:, 0:T], xs[sl][:, kk, 0:T], AF.Square, reads=[RXS[sl]], writes=[RSQ[q]])
                S.mm(ps[0][:, 0:T], ones[:], sq[q][:, 0:T], k == 0, k == 31, reads=[RSQ[q], R_const], writes=[RP[0]])
        S.ts(rstd[:, 0:T], ps[0][:, 0:T], 1.0 / D, EPS, ALU.mult, ALU.add, reads=[RP[0]], writes=[R_rstd])
        S.act(rstd[:, 0:T], rstd[:, 0:T], AF.Sqrt, reads=[R_rstd], writes=[R_rstd])
        S.recip(rstd[:, 0:T], rstd[:, 0:T], reads=[R_rstd], writes=[R_rstd])
        for kq in range(8):
            sl = kq % 2
            src_load(kq, xs[sl], RXS[sl])
            for kk in range(4):
                k = kq * 4 + kk
                S.tt(xs[sl][:, kk, 0:T], xs[sl][:, kk, 0:T], rstd[:, 0:T], ALU.mult, reads=[RXS[sl], R_rstd], writes=[RXS[sl]], eng="pool")
                S.ts(dst[:, k, 0:T], xs[sl][:, kk, 0:T], gs[:, k:k + 1], sh[:, k:k + 1], ALU.mult, ALU.add,
                     reads=[RXS[sl], R_mod], writes=[R_dst[0]])

    esA = ExitStack()
    sb = lambda n, sh, dt: esA.enter_context(nc.sbuf_tensor(n, sh, dt))
    xs = [sb("xs%d" % i, [128, 4, 512], F32) for i in range(2)]
    RXS = [Res("xs%d" % i) for i in range(2)]
    sq = [sb("sq%d" % i, [128, 512], F32R) for i in range(2)]
    RSQ = [Res("sq%d" % i) for i in range(2)]
    rstd = sb("rstd", [128, 512], F32)
    R_rstd = Res("rstd")
    hT = sb("hT", [128, 32, 512], F32R)
    R_hT = Res("hT")
    R_dst = [R_hT]
    ws = [sb("ws%d" % i, [128, 32, 256], F32R) for i in range(3)]
    RWS = [Res("ws%d" % i) for i in range(3)]
    stg = [sb("stg%d" % i, [128, 512], F32R) for i in range(4)]
    RSTG = [Res("stgA%d" % i) for i in range(4)]
    xTv = xT.rearrange("(k p) t -> p k t", p=128)
    vtokv = vtok
    wi = 0
    si = 0
    pi = 0
    for ti in range(5):
        if ti < 4:
            segs = [(HALO + 512 * ti, 0, 512)]
        else:
            segs = [(0, 0, 256), (NTOK + HALO, 256, 256)]

        def src_load(kq, slot, res, segs=segs):
            for (e0, o0, ln) in segs:
                S.dma(slot[:, :, o0:o0 + ln], xTv[:, kq * 4:(kq + 1) * 4, e0:e0 + ln], res.key, writes=[res])
        norm_tile(src_load, 512, hT, gs1, sh1)
        if ti < 4:
            cslots = list(range(80))
        else:
            cslots = list(range(8, 24)) + list(range(32, 48))
        for cs in cslots:
            c0 = cs * 256
            sl = wi % 3
            wi += 1
            for kq in range(2):
                S.dma(ws[sl][:, kq * 16:(kq + 1) * 16, :],
                      w_in[cs][:, kq * 4096:(kq + 1) * 4096].rearrange("p (k n) -> p k n", n=256), "ws%d" % sl, writes=[RWS[sl]])
            isv = 4096 <= c0 < 6144
            if isv:
                for sub in range(4):
                    b = 1 + pi % 4
                    pi += 1
                    for k in range(32):
                        S.mm(ps[b][:, 0:256], hT[:, k, sub * 128:(sub + 1) * 128], ws[sl][:, k, :], k == 0, k == 31,
                             reads=[R_hT, RWS[sl]], writes=[RP[b]])
                    st = si % 4
                    si += 1
                    S.cp(stg[st][:, 0:256], ps[b][:, 0:256], reads=[RP[b]], writes=[RSTG[st]], eng="act")
                    o = sub * 128
                    for (e0, o0, ln) in segs:
                        if o0 <= o < o0 + ln:
                            S.dma(vtokv[e0 + o - o0: e0 + o - o0 + 128, c0 - 4096: c0 - 4096 + 256], stg[st][:, 0:256],
                                  RSTG[st].key, reads=[RSTG[st]], q="act")
            else:
                for half in range(2):
                    b = 1 + pi % 4
                    pi += 1
                    for k in range(32):
                        S.mm(ps[b][:], ws[sl][:, k, half * 128:(half + 1) * 128], hT[:, k, :], k == 0, k == 31,
                             reads=[R_hT, RWS[sl]], writes=[RP[b]])
                    st = si % 4
                    si += 1
                    S.cp(stg[st][:], ps[b][:], reads=[RP[b]], writes=[RSTG[st]], eng="act")
                    r0 = c0 + half * 128
                    for (e0, o0, ln) in segs:
                        S.dma(featT[r0:r0 + 128, e0:e0 + ln], stg[st][:, o0:o0 + ln], RSTG[st].key, reads=[RSTG[st]], q="act")
    S.barrier()
    esA.close()
    if upto < 2:
        S.emit()
        return nc

    esB = ExitStack()
    sb = lambda n, sh, dt: esB.enter_context(nc.sbuf_tensor(n, sh, dt))
    qh = [sb("qh%d" % i, [128, 2048], F32R) for i in range(2)]
    kh = [sb("kh%d" % i, [128, NEXT], F32R) for i in range(2)]
    vh = [sb("vh%d" % i, [128, 20, 128], F32R) for i in range(2)]
    bgs = [sb("bg%d" % i, [128, 640], F32) for i in range(2)]
    b1s = [sb("b1%d" % i, [128, 640], F32) for i in range(2)]
    b14s = [sb("b14%d" % i, [128, 640], F32) for i in range(2)]
    bts = [sb("bt%d" % i, [128, 768], F32) for i in range(2)]
    bbs = [sb("bb%d" % i, [128, 768], F32) for i in range(2)]
    RQ = [Res("qh%d" % i) for i in range(2)]
    RK = [Res("kh%d" % i) for i in range(2)]
    RV = [Res("vh%d" % i) for i in range(2)]
    RB = [Res("bias%d" % i) for i in range(2)]
    sbt = [sb("sbt%d" % i, [128, 768], F32) for i in range(2)]
    Pn = [sb("Pn%d" % i, [128, 768], F32) for i in range(2)]
    PTs = [sb("PTs%d" % i, [128, 768], F32R) for i in range(2)]
    smx = [sb("smx%d" % i, [128, 4], F32) for i in range(2)]
    ah = [sb("ah%d" % i, [128, 2048], F32R) for i in range(2)]
    R_sbt = [Res("sbt%d" % i) for i in range(2)]
    R_Pn = [Res("Pn%d" % i) for i in range(2)]
    R_PT = [Res("PTs%d" % i) for i in range(2)]
    R_sm = [Res("smx%d" % i) for i in range(2)]
    RAH = [Res("ah%d" % i) for i in range(2)]
    vtv = vtok.rearrange("(n p) c -> p n c", p=128)
    it = 0
    for h in range(16):
        p = h % 2
        S.dma(qh[p][:], featT[h * 128:(h + 1) * 128, HALO:HALO + NTOK], RQ[p].key, writes=[RQ[p]])
        S.dma(kh[p][:], featT[2048 + h * 128:2048 + (h + 1) * 128, :], RK[p].key, writes=[RK[p]])
        S.dma(vh[p][:, 0:10, :], vtv[:, 0:10, h * 128:(h + 1) * 128], RV[p].key, writes=[RV[p]])
        S.dma(vh[p][:, 10:20, :], vtv[:, 10:20, h * 128:(h + 1) * 128], RV[p].key, writes=[RV[p]])
        S.dma(bgs[p][:], biasg[h], RB[p].key, writes=[RB[p]])
        S.dma(b1s[p][:], bias1[h], RB[p].key, writes=[RB[p]])
        S.dma(b14s[p][:], bias14[h], RB[p].key, writes=[RB[p]])
        S.dma(bts[p][:], biast[h], RB[p].key, writes=[RB[p]])
        S.dma(bbs[p][:], biasb[h], RB[p].key, writes=[RB[p]])
        for j in range(16):
            if j == 0:
                tiles, bias = list(range(0, 6)), bts[p]
            elif j == 15:
                tiles, bias = list(range(14, 20)), bbs[p]
            elif j == 1:
                tiles, bias = list(range(1, 6)), b1s[p]
            elif j == 14:
                tiles, bias = list(range(14, 19)), b14s[p]
            else:
                tiles, bias = list(range(j, j + 5)), bgs[p]
            nt = len(tiles)
            W = nt * 128
            e0 = tiles[0] * 128
            d = it % 2
            it += 1
            b0, b1_ = 2 + 2 * d, 3 + 2 * d
            qsl = qh[p][:, j * 128:(j + 1) * 128]
            S.mm(ps[b0][:, 0:512], qsl, kh[p][:, e0:e0 + 512], True, True, reads=[RQ[p], RK[p]], writes=[RP[b0]])
            S.mm(ps[b1_][:, 0:W - 512], qsl, kh[p][:, e0 + 512:e0 + W], True, True, reads=[RQ[p], RK[p]], writes=[RP[b1_]])
            S.stt(sbt[d][:, 0:512], ps[b0][:, 0:512], SCALE, bias[:, 0:512], ALU.mult, ALU.add,
                  reads=[RP[b0], RB[p]], writes=[R_sbt[d]])
            S.stt(sbt[d][:, 512:W], ps[b1_][:, 0:W - 512], SCALE, bias[:, 512:W], ALU.mult, ALU.add,
                  reads=[RP[b1_], RB[p]], writes=[R_sbt[d]])
            S.rmax(smx[d][:, 0:1], sbt[d][:, 0:W], reads=[R_sbt[d]], writes=[R_sm[d]])
            S.ts(smx[d][:, 1:2], smx[d][:, 0:1], -1.0, None, ALU.mult, reads=[R_sm[d]], writes=[R_sm[d]])
            S.act(Pn[d][:, 0:W], sbt[d][:, 0:W], AF.Exp, bias=smx[d][:, 1:2], accum_out=smx[d][:, 2:3],
                  reads=[R_sbt[d], R_sm[d]], writes=[R_Pn[d], R_sm[d]])
            S.recip(smx[d][:, 3:4], smx[d][:, 2:3], reads=[R_sm[d]], writes=[R_sm[d]])
            S.ts(Pn[d][:, 0:W], Pn[d][:, 0:W], smx[d][:, 3:4], None, ALU.mult, reads=[R_Pn[d], R_sm[d]], writes=[R_Pn[d]])
            for i in range(nt):
                bank = 6 if i < 4 else 7
                off = (i % 4) * 128
                S.tr(ps[bank][:, off:off + 128], Pn[d][:, i * 128:(i + 1) * 128], ident[:], reads=[R_Pn[d], R_const], writes=[RP[bank]])
            S.cp(PTs[d][:, 0:512], ps[6][:, 0:512], reads=[RP[6]], writes=[R_PT[d]], eng="act")
            S.cp(PTs[d][:, 512:W], ps[7][:, 0:W - 512], reads=[RP[7]], writes=[R_PT[d]], eng="act")
            for i in range(nt):
                S.mm(ps[1][:, 0:128], vh[p][:, tiles[i], :], PTs[d][:, i * 128:(i + 1) * 128], i == 0, i == nt - 1,
                     reads=[RV[p], R_PT[d]], writes=[RP[1]])
            S.cp(ah[p][:, j * 128:(j + 1) * 128], ps[1][:, 0:128], reads=[RP[1]], writes=[RAH[p]], eng="dve")
        S.dma(attnT[h * 128:(h + 1) * 128, :], ah[p][:], RAH[p].key, reads=[RAH[p]], q="act")
    S.barrier()
    esB.close()
    if upto < 3:
        S.emit()
        return nc

    esC = ExitStack()
    sb = lambda n, sh, dt: esC.enter_context(nc.sbuf_tensor(n, sh, dt))
    TB = 256
    aT = sb("aT", [128, 16, TB], F32R)
    cvT = sb("cvT", [128, 16, TB], F32R)
    mg = sb("mg", [128, 32, TB], F32R)
    wsl = [sb("wsl%d" % i, [128, 32, 256], F32R) for i in range(2)]
    RWS2 = [Res("wsl%d" % i) for i in range(2)]
    R_aT, R_cvT, R_mg = Res("aT"), Res("cvT"), Res("mg")
    cw = sb("cw", [128, 16, 3], F32)
    R_cw = Res("cw")
    S.dma(cw[:], convw, "cw", writes=[R_cw])
    cct = [sb("cct%d" % i, [128, TB + 2], F32) for i in range(2)]
    cht = [sb("cht%d" % i, [128, TB + 2], F32) for i in range(2)]
    cbt = [sb("cbt%d" % i, [128, TB], F32) for i in range(2)]
    ut = [sb("ut%d" % i, [128, TB + 2], F32) for i in range(2)]
    yt = [sb("yt%d" % i, [128, TB], F32) for i in range(2)]
    RCC = [Res("cct%d" % i) for i in range(2)]
    RCH = [Res("cht%d" % i) for i in range(2)]
    RCB = [Res("cbt%d" % i) for i in range(2)]
    RU = [Res("ut%d" % i) for i in range(2)]
    RY = [Res("yt%d" % i) for i in range(2)]
    gat = [sb("gat%d" % i, [128, TB], F32) for i in range(2)]
    gbt = [sb("gbt%d" % i, [128, TB], F32) for i in range(2)]
    t1 = [sb("t1%d" % i, [128, TB], F32) for i in range(2)]
    t2 = [sb("t2%d" % i, [128, TB], F32) for i in range(2)]
    RGA = [Res("gat%d" % i) for i in range(2)]
    RGB = [Res("gbt%d" % i) for i in range(2)]
    RT1 = [Res("t1%d" % i) for i in range(2)]
    RT2 = [Res("t2%d" % i) for i in range(2)]
    xm = [sb("xm%d" % i, [128, TB], F32) for i in range(2)]
    stg2 = [sb("stgB%d" % i, [128, TB], F32) for i in range(2)]
    RXM = [Res("xm%d" % i) for i in range(2)]
    RST2 = [Res("stgB%d" % i) for i in range(2)]
    attnTv = attnT.rearrange("(k p) t -> p k t", p=128)
    featF = featT.bitcast(F32)
    wi = 0
    pi = 0
    for ti in range(NTOK // TB):
        t0 = ti * TB
        e0 = HALO + t0
        S.dma(aT[:, 0:8, :], attnTv[:, 0:8, t0:t0 + TB], "aT", writes=[R_aT])
        S.dma(aT[:, 8:16, :], attnTv[:, 8:16, t0:t0 + TB], "aT", writes=[R_aT])
        for k in range(16):
            q = k % 2
            S.dma(cct[q][:], featF[8192 + k * 128:8192 + (k + 1) * 128, e0 - 1:e0 + TB + 1], RCC[q].key, writes=[RCC[q]], q="act")
            S.dma(cht[q][:], featF[10240 + k * 128:10240 + (k + 1) * 128, e0 - 1:e0 + TB + 1], RCH[q].key, writes=[RCH[q]], q="act")
            S.dma(cbt[q][:], featF[6144 + k * 128:6144 + (k + 1) * 128, e0:e0 + TB], RCB[q].key, writes=[RCB[q]], q="act")
            S.tt(ut[q][:], cct[q][:], cht[q][:], ALU.mult, reads=[RCC[q], RCH[q]], writes=[RU[q]], eng="pool")
            if ti == 0:
                S.ts(ut[q][:, 0:1], ut[q][:, 0:1], flags[:, 0:1], None, ALU.mult, reads=[RU[q], R_const], writes=[RU[q]], eng="pool")
            if ti == NTOK // TB - 1:
                S.ts(ut[q][:, TB + 1:TB + 2], ut[q][:, TB + 1:TB + 2], flags[:, 1:2], None, ALU.mult, reads=[RU[q], R_const], writes=[RU[q]], eng="pool")
            S.ts(yt[q][:], ut[q][:, 0:TB], cw[:, k, 0:1], None, ALU.mult, reads=[RU[q], R_cw], writes=[RY[q]])
            S.stt(yt[q][:], ut[q][:, 1:TB + 1], cw[:, k, 1:2], yt[q][:], ALU.mult, ALU.add, reads=[RU[q], R_cw, RY[q]], writes=[RY[q]])
            S.stt(yt[q][:], ut[q][:, 2:TB + 2], cw[:, k, 2:3], yt[q][:], ALU.mult, ALU.add, reads=[RU[q], R_cw, RY[q]], writes=[RY[q]])
            S.tt(cvT[:, k, :], cbt[q][:], yt[q][:], ALU.mult, reads=[RCB[q], RY[q]], writes=[R_cvT])
        for cs in range(16):
            c0 = cs * 256
            sl = wi % 2
            wi += 1
            S.dma(wsl[sl][:, 0:16, :], w_ao[cs].rearrange("p (k n) -> p k n", n=256), RWS2[sl].key, writes=[RWS2[sl]])
            S.dma(wsl[sl][:, 16:32, :], w_co[cs].rearrange("p (k n) -> p k n", n=256), RWS2[sl].key, writes=[RWS2[sl]])
            for half in range(2):
                m = cs * 2 + half
                ba, bb_ = (1, 2) if pi % 2 == 0 else (3, 4)
                pi += 1
                for k in range(16):
                    S.mm(ps[ba][:, 0:TB], wsl[sl][:, k, half * 128:(half + 1) * 128], aT[:, k, :], k == 0, k == 15,
                         reads=[RWS2[sl], R_aT], writes=[RP[ba]])
                for k in range(16):
                    S.mm(ps[bb_][:, 0:TB], wsl[sl][:, 16 + k, half * 128:(half + 1) * 128], cvT[:, k, :], k == 0, k == 15,
                         reads=[RWS2[sl], R_cvT], writes=[RP[bb_]])
                q = m % 2
                S.dma(gat[q][:], featF[12288 + m * 128:12288 + (m + 1) * 128, e0:e0 + TB], RGA[q].key, writes=[RGA[q]], q="act")
                S.dma(gbt[q][:], featF[16384 + m * 128:16384 + (m + 1) * 128, e0:e0 + TB], RGB[q].key, writes=[RGB[q]], q="act")
                S.act(gat[q][:], gat[q][:], AF.Sigmoid, reads=[RGA[q]], writes=[RGA[q]])
                S.act(gbt[q][:], gbt[q][:], AF.Sigmoid, reads=[RGB[q]], writes=[RGB[q]])
                S.tt(t1[q][:], ps[ba][:, 0:TB], gat[q][:], ALU.mult, reads=[RP[ba], RGA[q]], writes=[RT1[q]])
                S.tt(t2[q][:], ps[bb_][:, 0:TB], gbt[q][:], ALU.mult, reads=[RP[bb_], RGB[q]], writes=[RT2[q]])
                S.tt(mg[:, m, :], t1[q][:], t2[q][:], ALU.add, reads=[RT1[q], RT2[q]], writes=[R_mg])
        for cs in range(16):
            c0 = cs * 256
            sl = wi % 2
            wi += 1
            for kq in range(2):
                S.dma(wsl[sl][:, kq * 16:(kq + 1) * 16, :],
                      w_o[cs][:, kq * 4096:(kq + 1) * 4096].rearrange("p (k n) -> p k n", n=256), RWS2[sl].key, writes=[RWS2[sl]])
            for half in range(2):
                m = cs * 2 + half
                bo = 5 + m % 2
                for k in range(32):
                    S.mm(ps[bo][:, 0:TB], wsl[sl][:, k, half * 128:(half + 1) * 128], mg[:, k, :], k == 0, k == 31,
                         reads=[RWS2[sl], R_mg], writes=[RP[bo]])
                q = m % 2
                S.dma(xm[q][:], xT[m * 128:(m + 1) * 128, e0:e0 + TB], RXM[q].key, writes=[RXM[q]], q="act")
                S.stt(stg2[q][:], ps[bo][:, 0:TB], gate1[:, m:m + 1], xm[q][:], ALU.mult, ALU.add,
                      reads=[RP[bo], RXM[q], R_mod], writes=[RST2[q]])
                S.dma(x1T[m * 128:(m + 1) * 128, t0:t0 + TB], stg2[q][:], RST2[q].key, reads=[RST2[q]], q="act")
    S.barrier()
    esC.close()
    if upto < 4:
        S.emit()
        return nc

    esD = ExitStack()
    sb = lambda n, sh, dt: esD.enter_context(nc.sbuf_tensor(n, sh, dt))
    TC = 256
    NSB = 16
    accmem = sb("accmem", [128, 32 * TC], F32)
    acc = accmem[:].rearrange("p (k t) -> p k t", k=32)
    P_tok = [accmem[:, sub * 4096:(sub + 1) * 4096] for sub in range(2)]
    acc2 = accmem[:].rearrange("p (s d) -> p s d", s=2)
    xmc = [sb("xmc%d" % i, [128, TC], F32) for i in range(2)]
    RXMC = [Res("xmc%d" % i) for i in range(2)]
    RACC = [Res("acc%d" % m) for m in range(32)]
    h2T = sb("h2T", [128, 32, TC], F32R)
    R_h2 = Res("h2T")
    NUS, NVS = 3, 5
    us = [sb("us%d" % i, [128, 32, 128], F32R) for i in range(NUS)]
    RUS = [Res("us%d" % i) for i in range(NUS)]
    vs_ = [sb("vs%d" % i, [128, 2, 512], F32R) for i in range(NVS)]
    RVS = [Res("vs%d" % i) for i in range(NVS)]
    NTILE_C = NTOK // 256
    ust = {"next": 0, "use": 0}
    vst = {"next": 0, "use": 0}

    def u_prefetch(upto_i):
        while ust["next"] <= upto_i and ust["next"] < NTILE_C * 144:
            i = ust["next"]
            ust["next"] += 1
            r = i % 144
            src = w_q[r] if r < 16 else UT[r - 16]
            sl_ = i % NUS
            S.dma(us[sl_][:], src.rearrange("p (k n) -> p k n", n=128), RUS[sl_].key, writes=[RUS[sl_]])

    def u_use():
        i = ust["use"]
        ust["use"] += 1
        u_prefetch(i + NUS - 1)
        return i % NUS

    def v_prefetch(upto_j):
        while vst["next"] <= upto_j and vst["next"] < NTILE_C * 16 * 32:
            j = vst["next"]
            vst["next"] += 1
            sbk_, r_ = (j // 32) % 16, j % 32
            sl_ = j % NVS
            S.dma(vs_[sl_][:], EV[sbk_, r_ // 4, r_ % 4].rearrange("p (c n) -> p c n", n=512), RVS[sl_].key, writes=[RVS[sl_]])

    def v_use():
        j = vst["use"]
        vst["use"] += 1
        v_prefetch(j + NVS - 1)
        return j % NVS
    zbuf = sb("zbuf", [128, 4096], F32)
    qTs = zbuf[:].rearrange("p (g t) -> p g t", g=16)
    R_qT = Res("qTs")
    GAT = sb("GAT", [128, 8, TC], F32R)
    R_GAT = Res("GAT")
    s12 = [sb("s12%d" % i, [128, 16, 128], F32) for i in range(2)]
    R_s12 = [Res("s12%d" % i) for i in range(2)]
    gkmem = sb("gkmem", [128, 2048], F32)
    Gacc = [[sb("Gacc0_%d" % i, [128, 8, 128], F32)[:] for i in range(2)],
            [gkmem[:, i * 1024:(i + 1) * 1024].rearrange("p (c n) -> p c n", n=128) for i in range(2)]]
    R_G = [[Res("Gacc%d_%d" % (pp, i)) for i in range(2)] for pp in range(2)]
    zt = [zbuf[:, i * 1024:(i + 1) * 1024].rearrange("p (c n) -> p c n", n=128) for i in range(2)]
    ezt = [zbuf[:, (2 + i) * 1024:(3 + i) * 1024].rearrange("p (c n) -> p c n", n=128) for i in range(2)]
    R_z = [Res("zt%d" % i) for i in range(2)]
    R_ez = [Res("ezt%d" % i) for i in range(2)]
    AgT = [sb("AgT%d" % i, [128, TC], F32) for i in range(2)]
    R_Ag = [Res("AgT%d" % i) for i in range(2)]
    keys_sb = gkmem[:].rearrange("p (g n) -> p g n", n=128)
    R_keys = Res("keys")
    sq2 = [sb("sqc%d" % i, [128, TC], F32R) for i in range(2)]
    RSQ2 = [Res("sqc%d" % i) for i in range(2)]
    rstd2 = sb("rstd2", [128, TC], F32)
    R_rstd2 = Res("rstd2")
    ntmp = [sb("ntmp%d" % i, [128, TC], F32) for i in range(2)]
    RNT = [Res("ntmp%d" % i) for i in range(2)]
    vtop = [sb("vtop%d" % i, [128, 16, 16], F32) for i in range(2)]
    mrt = sb("mrt", [128, 256], F32)
    cand = zbuf[:, 0:2048].rearrange("p (h a b) -> p h a b", h=8, a=16)
    tsv = [sb("tsv%d" % i, [128, 8, 16], F32) for i in range(2)]
    sm2 = [sb("sm2%d" % i, [128, 4, 8], F32) for i in range(2)]
    extmp = sb("extmp", [128, 8, 16], F32)
    R_top = [Res("top%d" % i) for i in range(2)]
    R_mrt, R_cand, R_ext = Res("mrt"), Res("cand"), Res("extmp")
    stg3 = [sb("stgC%d" % i, [128, TC], F32) for i in range(2)]
    RST3 = [Res("stgC%d" % i) for i in range(2)]
    x1Tv = x1T.rearrange("(k p) t -> p k t", p=128)
    ui = 0
    vi = 0
    zi = 0
    ai = 0
    for ti in range(NTOK // TC):
        t0 = ti * TC
        S.dma(keys_sb, keysT, "keys", writes=[R_keys, R_G[1][0], R_G[1][1]])
        for kq in range(4):
            S.dma(acc[:, kq * 8:(kq + 1) * 8, :], x1Tv[:, kq * 8:(kq + 1) * 8, t0:t0 + TC], "accl%d" % kq,
                  writes=RACC[kq * 8:(kq + 1) * 8])
        for k in range(32):
            q = k % 2
            S.act(sq2[q][:], acc[:, k, :], AF.Square, reads=[RACC[k]], writes=[RSQ2[q]])
            S.mm(ps[0][:, 0:TC], ones[:], sq2[q][:], k == 0, k == 31, reads=[RSQ2[q], R_const], writes=[RP[0]])
        S.ts(rstd2[:], ps[0][:, 0:TC], 1.0 / D, EPS, ALU.mult, ALU.add, reads=[RP[0]], writes=[R_rstd2])
        S.act(rstd2[:], rstd2[:], AF.Sqrt, reads=[R_rstd2], writes=[R_rstd2])
        S.recip(rstd2[:], rstd2[:], reads=[R_rstd2], writes=[R_rstd2])
        for k in range(32):
            q = k % 2
            S.tt(ntmp[q][:], acc[:, k, :], rstd2[:], ALU.mult, reads=[RACC[k], R_rstd2], writes=[RNT[q]], eng="pool")
            S.ts(h2T[:, k, :], ntmp[q][:], gs2[:, k:k + 1], sh2[:, k:k + 1], ALU.mult, ALU.add,
                 reads=[RNT[q], R_mod], writes=[R_h2])
        for g in range(16):
            sl = u_use()
            bq = 1 + g % 2
            for k in range(32):
                S.mm(ps[bq][:, 0:TC], us[sl][:, k, :], h2T[:, k, :], k == 0, k == 31, reads=[RUS[sl], R_h2], writes=[RP[bq]])
            S.cp(qTs[:, g, :], ps[bq][:, 0:TC], reads=[RP[bq]], writes=[R_qT, R_z[0], R_z[1], R_ez[0], R_ez[1]], eng=("act" if g % 2 else "dve"))
        for u in range(2):
            for gq in range(4):
                bank = 3 + gq % 2
                for gg in range(4):
                    g = gq * 4 + gg
                    S.mm(ps[bank][:, gg * 128:(gg + 1) * 128], qTs[:, g, u * 128:(u + 1) * 128], keys_sb[:, g, :], True, True,
                         reads=[R_qT, R_z[0], R_z[1], R_ez[0], R_ez[1], R_keys, R_G[1][0], R_G[1][1]], writes=[RP[bank]])
                S.cp(s12[u][:, gq * 4:(gq + 1) * 4, :], ps[bank][:, 0:512].rearrange("p (a b) -> p a b", a=4),
                     reads=[RP[bank]], writes=[R_s12[u]], eng="act")
        for u in range(2):
            for g in range(16):
                S.max8(vtop[u][:, g, 0:8], s12[u][:, g, :], reads=[R_s12[u]], writes=[R_top[u]])
                S.mrep(mrt[:, 0:128], vtop[u][:, g, 0:8], s12[u][:, g, :], reads=[R_s12[u], R_top[u]], writes=[R_mrt])
                S.max8(vtop[u][:, g, 8:16], mrt[:, 0:128], reads=[R_mrt], writes=[R_top[u]])
            v1 = vtop[u][:, 0:16:2, :].unsqueeze(3).to_broadcast([128, 8, 16, 16])
            v2 = vtop[u][:, 1:16:2, :].unsqueeze(2).to_broadcast([128, 8, 16, 16])
            S.tt(cand, v1, v2, ALU.add, reads=[R_top[u]], writes=[R_cand, R_z[0], R_z[1], R_qT])
            for hh in range(8):
                cflat = cand[:, hh, :, :].rearrange("p a b -> p (a b)")
                S.max8(tsv[u][:, hh, 0:8], cflat, reads=[R_cand, R_z[0], R_z[1]], writes=[R_top[u]])
                S.mrep(mrt[:], tsv[u][:, hh, 0:8], cflat, reads=[R_cand, R_z[0], R_z[1], R_top[u]], writes=[R_mrt])
                S.max8(tsv[u][:, hh, 8:16], mrt[:], reads=[R_mrt], writes=[R_top[u]])
            S.ts(sm2[u][:, 0, :], tsv[u][:, :, 0], -1.0, None, ALU.mult, reads=[R_top[u]], writes=[R_top[u]])
            S.cp(sm2[u][:, 1, :], tsv[u][:, :, 15], reads=[R_top[u]], writes=[R_top[u]])
            S.tt(extmp[:], tsv[u][:], sm2[u][:, 0, :].unsqueeze(2).to_broadcast([128, 8, 16]), ALU.add,
                 reads=[R_top[u]], writes=[R_ext])
            S.act(extmp[:], extmp[:], AF.Exp, reads=[R_ext], writes=[R_ext])
            S.rsum(sm2[u][:, 2, :], extmp[:], reads=[R_ext], writes=[R_top[u]])
            S.recip(sm2[u][:, 3, :], sm2[u][:, 2, :], reads=[R_top[u]], writes=[R_top[u]])
        def G_iter(sbk_, u, hh):
            nonlocal zi
            cb_ = sbk_ * 8
            zb = zi % 2
            zi += 1
            s2b = s12[u][:, 2 * hh + 1, :].unsqueeze(1).to_broadcast([128, 8, 128])
            s1b = s12[u][:, 2 * hh, cb_:cb_ + 8].unsqueeze(2).to_broadcast([128, 8, 128])
            S.tt(zt[zb][:], s2b, s1b, ALU.add, reads=[R_s12[u]], writes=[R_z[zb]], eng="pool")
            S.act(ezt[zb][:], zt[zb][:], AF.Exp, bias=sm2[u][:, 0, hh:hh + 1], reads=[R_z[zb], R_top[u]], writes=[R_ez[zb]])
            S.stt(ezt[zb][:], zt[zb][:], sm2[u][:, 1, hh:hh + 1], ezt[zb][:], ALU.is_ge, ALU.mult,
                  reads=[R_z[zb], R_ez[zb], R_top[u]], writes=[R_ez[zb]])
            gp = sbk_ % 2
            if hh == 0:
                S.ts(Gacc[gp][u], ezt[zb][:], sm2[u][:, 3, hh:hh + 1], None, ALU.mult,
                     reads=[R_ez[zb], R_top[u]], writes=[R_G[gp][u]])
            else:
                S.stt(Gacc[gp][u], ezt[zb][:], sm2[u][:, 3, hh:hh + 1], Gacc[gp][u], ALU.mult, ALU.add,
                      reads=[R_ez[zb], R_top[u], R_G[gp][u]], writes=[R_G[gp][u]])

        for u in range(2):
            for hh in range(8):
                G_iter(0, u, hh)
        for sbk in range(NSB):
            v_prefetch(((ti * NSB) + sbk) * 32 + NVS - 1)
            giters = [(u, hh) for u in range(2) for hh in range(8)] if sbk + 1 < NSB else []
            for cc in range(8):
                sl = u_use()
                ba = 1 + ai % 2
                aq = ai % 2
                ai += 1
                for k in range(32):
                    S.mm(ps[ba][:, 0:TC], us[sl][:, k, :], h2T[:, k, :], k == 0, k == 31, reads=[RUS[sl], R_h2], writes=[RP[ba]])
                S.act(AgT[aq][:], ps[ba][:, 0:TC], AF.Gelu_apprx_tanh, reads=[RP[ba]], writes=[R_Ag[aq]])
                for u in range(2):
                    S.tr(ps[5][:, u * 128:(u + 1) * 128], Gacc[sbk % 2][u][:, cc, :], ident[:], reads=[R_G[sbk % 2][u], R_const], writes=[RP[5]])
                S.tt(GAT[:, cc, :], AgT[aq][:], ps[5][:, 0:TC], ALU.mult, reads=[R_Ag[aq], RP[5]], writes=[R_GAT])
                if giters:
                    G_iter(sbk + 1, *giters.pop(0))
            for dg in range(8):
                banks = (6, 7) if dg % 2 == 0 else (3, 4)
                for ccp in range(4):
                    sl = v_use()
                    for c2 in range(2):
                        cc = ccp * 2 + c2
                        for sub in range(2):
                            S.mm(ps[banks[sub]][:], GAT[:, cc, sub * 128:(sub + 1) * 128], vs_[sl][:, c2, :], cc == 0, cc == 7,
                                 reads=[R_GAT, RVS[sl]], writes=[RP[banks[sub]]])
                for sub in range(2):
                    rr = [RACC[sub * 16 + 2 * dg], RACC[sub * 16 + 2 * dg + 1]]
                    dst = P_tok[sub][:, dg * 512:(dg + 1) * 512]
                    if sbk == 0:
                        S.cp(dst, ps[banks[sub]][:], reads=[RP[banks[sub]]], writes=rr)
                    else:
                        S.tt(dst, ps[banks[sub]][:], dst, ALU.add, reads=[RP[banks[sub]]] + rr, writes=rr)
                if giters:
                    G_iter(sbk + 1, *giters.pop(0))
        for m in range(32):
            q = m % 2
            S.dma(xmc[q][:], x1T[m * 128:(m + 1) * 128, t0:t0 + TC], RXMC[q].key, writes=[RXMC[q]], q="act")
            rr = [RACC[m // 2], RACC[16 + m // 2]]
            for sub in range(2):
                S.tr(ps[5][:, sub * 128:(sub + 1) * 128], P_tok[sub][:, m * 128:(m + 1) * 128], ident[:], reads=[rr[sub], R_const], writes=[RP[5]])
            x2v = acc2[:, :, m * 128:(m + 1) * 128]
            S.stt(x2v, ps[5][:, 0:TC].rearrange("p (s t) -> p s t", s=2), gate2[:, m:m + 1],
                  xmc[q][:].rearrange("p (s t) -> p s t", s=2), ALU.mult, ALU.add,
                  reads=[RP[5], RXMC[q], R_mod] + rr, writes=rr)
            S.act(sq2[q][:].rearrange("p (s t) -> p s t", s=2), x2v, AF.Square, reads=rr, writes=[RSQ2[q]])
            S.mm(ps[0][:, 0:TC], ones[:], sq2[q][:], m == 0, m == 31, reads=[RSQ2[q], R_const], writes=[RP[0]])
        S.ts(rstd2[:], ps[0][:, 0:TC], 1.0 / D, EPS, ALU.mult, ALU.add, reads=[RP[0]], writes=[R_rstd2])
        S.act(rstd2[:], rstd2[:], AF.Sqrt, reads=[R_rstd2], writes=[R_rstd2])
        S.recip(rstd2[:], rstd2[:], reads=[R_rstd2], writes=[R_rstd2])
        for m in range(32):
            q = m % 2
            rr = [RACC[m // 2], RACC[16 + m // 2]]
            x2v = acc2[:, :, m * 128:(m + 1) * 128]
            S.tt(ntmp[q][:].rearrange("p (s t) -> p s t", s=2), x2v, rstd2[:].rearrange("p (s t) -> p s t", s=2), ALU.mult,
                 reads=rr + [R_rstd2], writes=[RNT[q]], eng="pool")
            S.ts(stg3[q][:], ntmp[q][:], gfs[:, m:m + 1], None, ALU.mult, reads=[RNT[q], R_const], writes=[RST3[q]])
            S.dma(outT[m * 128:(m + 1) * 128, t0:t0 + TC], stg3[q][:], RST3[q].key, reads=[RST3[q]], q="act")
    S.barrier()
    esD.close()
    S.emit()
    return nc


def _bias_tables(rpb_l, top_edge, bot_edge):
    H = 16
    qc = np.arange(64)
    cstart = np.clip(qc - 8, 0, 48)

    def table(width_rows, row_off, qrows_abs, rs_abs):
        W = width_rows * 64
        tab = np.full((H, 128, W), NEG, np.float32)
        for qr in range(2):
            rs = rs_abs[qr]
            for i in range(8):
                krow = rs + i
                wr = krow - row_off
                if wr < 0 or wr >= width_rows:
                    continue
                dr = krow - qr + 7
                for x in range(64):
                    ys = cstart[x] + np.arange(16)
                    dc = ys - x + 15
                    tab[:, qr * 64 + x, wr * 64 + ys] = rpb_l[:, dr, dc]
        return tab

    gen = table(10, -4, None, (-4, -3))
    if top_edge:
        top = table(12, -4, None, (0, 0))
    else:
        top = table(12, -4, None, (-4, -3))
    if bot_edge:
        bot = table(12, -6, None, (-6, -6))
    else:
        bot = table(12, -6, None, (-4, -3))
    top1 = table(10, -4, None, (-2, -2)) if top_edge else gen
    bot14 = table(10, -4, None, (-4, -4)) if bot_edge else gen
    return gen, top, bot, top1, bot14


_NC_CACHE = {}


def _prep_inputs(inp):
    x = np.asarray(inp["x"], np.float32)
    c = np.asarray(inp["c"], np.float32)
    l = 0
    shared = {
        "w_ada": np.ascontiguousarray(inp["w_ada"][l], np.float32),
        "b_ada": np.ascontiguousarray(inp["b_ada"][l].reshape(1, -1), np.float32),
        "g1": np.ascontiguousarray(inp["norm1_g"][l].reshape(32, 128).T, np.float32),
        "g2": np.ascontiguousarray(inp["norm2_g"][l].reshape(32, 128).T, np.float32),
        "gf": np.ascontiguousarray(np.asarray(inp["norm_f_g"]).reshape(32, 128).T, np.float32),
        "w_in": np.ascontiguousarray(np.asarray(inp["w_in"][l], np.float32).reshape(32, 128, 80, 256).transpose(2, 1, 0, 3)).reshape(80, 128, 8192),
        "convw": np.ascontiguousarray(np.asarray(inp["conv_w"][l]).reshape(3, 16, 128).transpose(2, 1, 0), np.float32),
        "w_ao": np.ascontiguousarray(np.asarray(inp["w_attn_out"][l], np.float32).reshape(16, 128, 16, 256).transpose(2, 1, 0, 3)).reshape(16, 128, 4096),
        "w_co": np.ascontiguousarray(np.asarray(inp["w_conv_out"][l], np.float32).reshape(16, 128, 16, 256).transpose(2, 1, 0, 3)).reshape(16, 128, 4096),
        "w_o": np.ascontiguousarray(np.asarray(inp["w_o"][l], np.float32).reshape(32, 128, 16, 256).transpose(2, 1, 0, 3)).reshape(16, 128, 8192),
        "w_q": np.ascontiguousarray(np.asarray(inp["w_q_peer"][l], np.float32).reshape(32, 128, 16, 128).transpose(2, 1, 0, 3)).reshape(16, 128, 4096),
        "UT": np.ascontiguousarray(np.asarray(inp["expert_u"][l], np.float32).reshape(128, 128, 32, 128).transpose(0, 3, 2, 1)).reshape(128, 128, 4096),
        "EV": np.ascontiguousarray(np.asarray(inp["expert_v"][l], np.float32).reshape(16, 4, 2, 128, 8, 512).transpose(0, 4, 1, 3, 2, 5)).reshape(16, 8, 4, 128, 1024),
        "ident": np.eye(128, dtype=np.float32),
    }
    k1 = np.asarray(inp["sub_keys_1"][l])
    k2 = np.asarray(inp["sub_keys_2"][l])
    kk = np.stack([k1, k2], axis=1).reshape(16, 128, 128)
    shared["keysT"] = np.ascontiguousarray(kk.transpose(2, 0, 1), np.float32)
    rpb_l = np.asarray(inp["rpb"][l], np.float32)
    tabs = {}
    maps = []
    for core in range(8):
        b, qd = core // 4, core % 4
        t0 = qd * NTOK
        xe = np.zeros((NEXT, D), np.float32)
        lo, hi = t0 - HALO, t0 + NTOK + HALO
        slo, shi = max(lo, 0), min(hi, 8192)
        xe[slo - lo: shi - lo] = x[b, slo:shi]
        m = dict(shared)
        m["xT"] = np.ascontiguousarray(xe.T)
        m["cT"] = np.ascontiguousarray(c[b].reshape(32, 128).T, np.float32)
        te, be = (qd == 0), (qd == 3)
        if (te, be) not in tabs:
            tabs[(te, be)] = _bias_tables(rpb_l, te, be)
        g_, t_, b_, t1_, b14_ = tabs[(te, be)]
        m["biasg"], m["biast"], m["biasb"], m["bias1"], m["bias14"] = g_, t_, b_, t1_, b14_
        fl = np.ones((128, 2), np.float32)
        if te:
            fl[:, 0] = 0.0
        if be:
            fl[:, 1] = 0.0
        m["flags"] = fl
        maps.append(m)
    return maps


def kernel(**inputs):
    maps = _prep_inputs(inputs)
    if "nc" not in _NC_CACHE:
        _NC_CACHE["nc"] = build_nc()
    nc = _NC_CACHE["nc"]
    res = run_bass_kernel_spmd(nc, maps, core_ids=list(range(8)))
    out = np.zeros((2, 8192, D), np.float32)
    for core in range(8):
        b, qd = core // 4, core % 4
        out[b, qd * NTOK:(qd + 1) * NTOK] = res.results[core]["outT"].T
    return out
```
